# Optimizing a Trainium2 kernel written in Bass

```python
import math
import jax, jax.numpy as jnp
from jax import lax
import numpy as np

D_MODEL = 1024
BATCH = 16
SEQ = 2048
DEPTH = 1

D_ATTN = D_MODEL // 2
HEAD_DIM_A = 64
N_HEADS_A = D_ATTN // HEAD_DIM_A
DILATED_BRANCHES = ((128, 1), (512, 4), (2048, 16))
BAND_BLOCK = 128

D_MLSTM = D_MODEL // 2
N_HEADS_M = 4
HEAD_DIM_M = D_MLSTM // N_HEADS_M
CONV_WIDTH = 4
MLSTM_CHUNK = 128

D_MIX = D_ATTN + D_MLSTM
PROJ_WIDTHS = (D_ATTN, D_ATTN, D_ATTN, D_MLSTM, D_MLSTM, D_MLSTM, N_HEADS_M, N_HEADS_M)

N_EXPERTS = 32
TOP_K = 4
D_EXPERT = D_MODEL
SWIGLU_LIMIT = 7.0
SWIGLU_ALPHA = 1.702
MOE_BLOCK = 128

DEEPNORM_ALPHA = (2 * DEPTH) ** 0.25
DEEPNORM_BETA = (8 * DEPTH) ** -0.25
LN_EPS = 1e-5
RMS_EPS = 1e-6

kernel_name = "hymba_dilated_mlstm_moe_deepnorm"


def _layer_norm(x, g, b):
    xf = x.astype(jnp.float32)
    mu = jnp.mean(xf, axis=-1, keepdims=True)
    var = jnp.mean(jnp.square(xf - mu), axis=-1, keepdims=True)
    return ((xf - mu) * lax.rsqrt(var + LN_EPS) * g + b).astype(x.dtype)


def _split_heads(t, n_heads):
    b, s, _ = t.shape
    return t.reshape(b, s, n_heads, -1).transpose(0, 2, 1, 3)


def _dilated_branch(q, k, v, window, dilation):
    b, h, s, e = q.shape
    n = s // dilation
    span = window // dilation
    L = BAND_BLOCK
    n_pad = -(-n // L) * L
    nb = n_pad // L

    def to_sub(t):
        t = t.reshape(b, h, n, dilation, e).transpose(0, 1, 3, 2, 4)
        t = jnp.pad(t, ((0, 0), (0, 0), (0, 0), (0, n_pad - n), (0, 0)))
        return t.reshape(b, h, dilation, nb, L, e)

    def with_prev(t):
        prev = jnp.pad(t[:, :, :, :-1], ((0, 0), (0, 0), (0, 0), (1, 0), (0, 0), (0, 0)))
        return jnp.concatenate([prev, t], axis=4)

    qs = to_sub(q)
    kb = with_prev(to_sub(k))
    vb = with_prev(to_sub(v))
    scores = jnp.einsum('bhrnqe,bhrnke->bhrnqk', qs, kb).astype(jnp.float32) * (e ** -0.5)
    qi = jnp.arange(L)[:, None]
    kj = jnp.arange(2 * L)[None, :]
    dist = qi - kj + L
    key_pos = jnp.arange(nb)[:, None, None] * L + kj[None] - L
    mask = (dist >= 0) & (dist <= span) & (key_pos >= 0)
    scores = jnp.where(mask, scores, -jnp.inf)
    m = jnp.max(scores, axis=-1, keepdims=True)
    p = jnp.exp(scores - m)
    l = jnp.sum(p, axis=-1, keepdims=True)
    o = jnp.einsum('bhrnqk,bhrnke->bhrnqe', (p / l).astype(v.dtype), vb)
    lse = (m + jnp.log(l))[..., 0]
    o = o.reshape(b, h, dilation, n_pad, e)[:, :, :, :n]
    o = o.transpose(0, 1, 3, 2, 4).reshape(b, h, s, e)
    lse = lse.reshape(b, h, dilation, n_pad)[:, :, :, :n]
    lse = lse.transpose(0, 1, 3, 2).reshape(b, h, s)
    return o, lse


def _dilated_attention(q, k, v):
    outs, lses = [], []
    for window, dilation in DILATED_BRANCHES:
        o, lse = _dilated_branch(q, k, v, window, dilation)
        outs.append(o)
        lses.append(lse)
    wts = jax.nn.softmax(jnp.stack(lses, axis=0), axis=0)
    out = jnp.einsum('cbhs,cbhse->bhse', wts, jnp.stack(outs, axis=0).astype(jnp.float32))
    return out.astype(q.dtype)


def _causal_depthwise_conv(x, w, b):
    width, c = w.shape
    y = lax.conv_general_dilated(x, w[:, None, :].astype(x.dtype), window_strides=(1,),
                                 padding=((width - 1, 0),),
                                 dimension_numbers=('NWC', 'WIO', 'NWC'),
                                 feature_group_count=c)
    return y + b


def _mlstm_chunkwise(q, k, v, i_pre, f_pre):
    b, h, s, e = q.shape
    L = MLSTM_CHUNK
    nc = s // L
    q = q.astype(jnp.float32) * (e ** -0.5)
    k = k.astype(jnp.float32)
    v = v.astype(jnp.float32)
    i_pre = i_pre.astype(jnp.float32)
    log_f = jax.nn.log_sigmoid(f_pre.astype(jnp.float32))

    def chunks(t):
        return jnp.moveaxis(t.reshape(b, h, nc, L, *t.shape[3:]), 2, 0)

    causal = jnp.tril(jnp.ones((L, L), dtype=bool))

    def step(carry, inp):
        C, n, m = carry
        qc, kc, vc, ic, fc = inp
        g = jnp.cumsum(fc, axis=-1)
        D = g[..., :, None] - g[..., None, :] + ic[..., None, :]
        D = jnp.where(causal, D, -jnp.inf)
        inter = g + m[..., None]
        m_t = jnp.maximum(inter, jnp.max(D, axis=-1))
        w_intra = jnp.exp(D - m_t[..., None])
        w_inter = jnp.exp(inter - m_t)
        qk = jnp.einsum('bhqe,bhke->bhqk', qc, kc) * w_intra
        num = jnp.einsum('bhqk,bhkf->bhqf', qk, vc) + w_inter[..., None] * jnp.einsum('bhqe,bhef->bhqf', qc, C)
        den = jnp.sum(qk, axis=-1) + w_inter * jnp.einsum('bhqe,bhe->bhq', qc, n)
        hc = num / jnp.maximum(jnp.abs(den), jnp.exp(-m_t))[..., None]
        g_last = g[..., -1]
        a = g_last[..., None] - g + ic
        m_new = jnp.maximum(g_last + m, jnp.max(a, axis=-1))
        decay = jnp.exp(g_last + m - m_new)
        wk = jnp.exp(a - m_new[..., None])
        C_new = decay[..., None, None] * C + jnp.einsum('bhl,bhle,bhlf->bhef', wk, kc, vc)
        n_new = decay[..., None] * n + jnp.einsum('bhl,bhle->bhe', wk, kc)
        return (C_new, n_new, m_new), hc

    init = (jnp.zeros((b, h, e, e), jnp.float32), jnp.zeros((b, h, e), jnp.float32),
            jnp.zeros((b, h), jnp.float32))
    _, hs = lax.scan(step, init, (chunks(q), chunks(k), chunks(v), chunks(i_pre), chunks(log_f)))
    return jnp.moveaxis(hs, 0, 2).reshape(b, h, s, e)


def _mixer(x, w_in, conv_w, conv_b, w_mq, w_mk, b_igate, b_fgate, mnorm_g, w_out):
    b, s, _ = x.shape
    proj = x @ w_in
    split_at = np.cumsum(PROJ_WIDTHS)[:-1].tolist()
    qa, ka, va, xm, vm, om, ip, fp = jnp.split(proj, split_at, axis=-1)
    attn = _dilated_attention(_split_heads(qa, N_HEADS_A), _split_heads(ka, N_HEADS_A),
                              _split_heads(va, N_HEADS_A))
    attn = attn.transpose(0, 2, 1, 3).reshape(b, s, D_ATTN)
    xc = jax.nn.silu(_causal_depthwise_conv(xm, conv_w, conv_b))
    xc = _split_heads(xc, N_HEADS_M)
    qm = jnp.einsum('bhse,hef->bhsf', xc, w_mq)
    km = jnp.einsum('bhse,hef->bhsf', xc, w_mk)
    i_pre = (ip + b_igate).transpose(0, 2, 1)
    f_pre = (fp + b_fgate).transpose(0, 2, 1)
    hm = _mlstm_chunkwise(qm, km, _split_heads(vm, N_HEADS_M), i_pre, f_pre)
    hm = hm * lax.rsqrt(jnp.mean(jnp.square(hm), axis=-1, keepdims=True) + RMS_EPS)
    hm = hm * mnorm_g.reshape(N_HEADS_M, 1, HEAD_DIM_M)
    hm = hm.transpose(0, 2, 1, 3).reshape(b, s, D_MLSTM)
    hm = (jax.nn.sigmoid(om.astype(jnp.float32)) * hm).astype(x.dtype)
    return jnp.concatenate([attn, hm], axis=-1) @ w_out


def _moe(x2d, w_router, b_router, w_gate, b_gate, w_up, b_up, w_down, b_down):
    t, d = x2d.shape
    logits = (x2d @ w_router + b_router).astype(jnp.float32)
    top_val, top_idx = lax.top_k(logits, TOP_K)
    gate = jax.nn.softmax(top_val, axis=-1)
    n_assign = t * TOP_K
    flat_e = top_idx.reshape(-1)
    flat_tok = jnp.arange(n_assign, dtype=jnp.int32) // TOP_K
    flat_gate = gate.reshape(-1)
    order = jnp.argsort(flat_e)
    sorted_e = flat_e[order]
    counts = jnp.bincount(flat_e, length=N_EXPERTS)
    starts = jnp.cumsum(counts) - counts
    padded = (counts + MOE_BLOCK - 1) // MOE_BLOCK * MOE_BLOCK
    pends = jnp.cumsum(padded)
    pstarts = pends - padded
    dest = pstarts[sorted_e] + (jnp.arange(n_assign) - starts[sorted_e])
    n_rows = (-(-n_assign // MOE_BLOCK) + N_EXPERTS) * MOE_BLOCK
    n_blocks = n_rows // MOE_BLOCK
    row_tok = jnp.zeros((n_rows,), jnp.int32).at[dest].set(flat_tok[order])
    row_gate = jnp.zeros((n_rows,), jnp.float32).at[dest].set(flat_gate[order])
    block_e = jnp.minimum(jnp.searchsorted(pends, jnp.arange(n_blocks) * MOE_BLOCK, side='right'),
                          N_EXPERTS - 1)

    def expert_block(args):
        tok, e = args
        xb = x2d[tok]
        g = jnp.minimum(xb @ w_gate[e] + b_gate[e], SWIGLU_LIMIT)
        u = jnp.clip(xb @ w_up[e] + b_up[e], -SWIGLU_LIMIT, SWIGLU_LIMIT)
        hdn = g * jax.nn.sigmoid(SWIGLU_ALPHA * g) * (u + 1)
        return hdn @ w_down[e] + b_down[e]

    y = lax.map(expert_block, (row_tok.reshape(n_blocks, MOE_BLOCK), block_e))
    y = y.reshape(n_rows, d) * row_gate[:, None].astype(y.dtype)
    return jnp.zeros_like(x2d).at[row_tok].add(y)


def setup_inputs(seed: int = 0) -> dict:
    key = jax.random.key(seed)
    ks = jax.random.split(key, 23)

    def nrm(k, shape, scale):
        return jax.random.normal(k, shape, jnp.float32) * scale

    col_scale = jnp.concatenate([
        jnp.ones((2 * D_ATTN,), jnp.float32),
        jnp.full((D_ATTN,), DEEPNORM_BETA, jnp.float32),
        jnp.ones((D_MLSTM,), jnp.float32),
        jnp.full((D_MLSTM,), DEEPNORM_BETA, jnp.float32),
        jnp.ones((D_MLSTM + 2 * N_HEADS_M,), jnp.float32)])
    p_total = sum(PROJ_WIDTHS)
    return {
        "x": nrm(ks[0], (BATCH, SEQ, D_MODEL), 1.0),
        "w_in": nrm(ks[1], (DEPTH, D_MODEL, p_total), D_MODEL ** -0.5) * col_scale,
        "conv_w": nrm(ks[2], (DEPTH, CONV_WIDTH, D_MLSTM), CONV_WIDTH ** -0.5),
        "conv_b": nrm(ks[3], (DEPTH, D_MLSTM), 0.01),
        "w_mq": nrm(ks[4], (DEPTH, N_HEADS_M, HEAD_DIM_M, HEAD_DIM_M), HEAD_DIM_M ** -0.5),
        "w_mk": nrm(ks[5], (DEPTH, N_HEADS_M, HEAD_DIM_M, HEAD_DIM_M), HEAD_DIM_M ** -0.5),
        "b_igate": nrm(ks[6], (DEPTH, N_HEADS_M), 0.1),
        "b_fgate": jnp.linspace(3.0, 6.0, N_HEADS_M, dtype=jnp.float32)[None] + nrm(ks[7], (DEPTH, N_HEADS_M), 0.1),
        "mnorm_g": 1.0 + nrm(ks[8], (DEPTH, D_MLSTM), 0.02),
        "w_out": nrm(ks[9], (DEPTH, D_MIX, D_MODEL), D_MIX ** -0.5 * DEEPNORM_BETA),
        "ln1_g": 1.0 + nrm(ks[10], (DEPTH, D_MODEL), 0.02),
        "ln1_b": nrm(ks[11], (DEPTH, D_MODEL), 0.02),
        "w_router": nrm(ks[12], (DEPTH, D_MODEL, N_EXPERTS), D_MODEL ** -0.5),
        "b_router": nrm(ks[13], (DEPTH, N_EXPERTS), 0.01),
        "w_gate": nrm(ks[14], (DEPTH, N_EXPERTS, D_MODEL, D_EXPERT), D_MODEL ** -0.5),
        "b_gate": nrm(ks[15], (DEPTH, N_EXPERTS, D_EXPERT), 0.01),
        "w_up": nrm(ks[16], (DEPTH, N_EXPERTS, D_MODEL, D_EXPERT), D_MODEL ** -0.5 * DEEPNORM_BETA),
        "b_up": nrm(ks[17], (DEPTH, N_EXPERTS, D_EXPERT), 0.01),
        "w_down": nrm(ks[18], (DEPTH, N_EXPERTS, D_EXPERT, D_MODEL), D_EXPERT ** -0.5 * DEEPNORM_BETA),
        "b_down": nrm(ks[19], (DEPTH, N_EXPERTS, D_MODEL), 0.01),
        "ln2_g": 1.0 + nrm(ks[20], (DEPTH, D_MODEL), 0.02),
        "ln2_b": nrm(ks[21], (DEPTH, D_MODEL), 0.02),
    }


def reference(x, w_in, conv_w, conv_b, w_mq, w_mk, b_igate, b_fgate, mnorm_g, w_out,
              ln1_g, ln1_b, w_router, b_router, w_gate, b_gate, w_up, b_up, w_down, b_down,
              ln2_g, ln2_b):
    h = x
    for l in range(DEPTH):
        y = _mixer(h, w_in[l], conv_w[l], conv_b[l], w_mq[l], w_mk[l], b_igate[l], b_fgate[l],
                   mnorm_g[l], w_out[l])
        h = _layer_norm(DEEPNORM_ALPHA * h + y, ln1_g[l], ln1_b[l])
        y = _moe(h.reshape(-1, D_MODEL), w_router[l], b_router[l], w_gate[l], b_gate[l],
                 w_up[l], b_up[l], w_down[l], b_down[l]).reshape(h.shape)
        h = _layer_norm(DEEPNORM_ALPHA * h + y, ln2_g[l], ln2_b[l])
    return h
```

```python
import contextlib
import numpy as np
import concourse.bass as bass
import concourse.mybir as mybir
from concourse.bass_utils import run_bass_kernel_spmd

F32 = mybir.dt.float32
BF16 = mybir.dt.bfloat16
I32 = mybir.dt.int32
U32 = mybir.dt.uint32
AF = mybir.ActivationFunctionType
ALU = mybir.AluOpType
AX = mybir.AxisListType

ENGS = ("pe", "act", "dve", "pool", "sp")


class _Op:
    __slots__ = ("eng", "fn", "reads", "writes", "dma", "key", "idx", "deps",
                 "need_inc", "cnt", "sem")

    def __init__(self, eng, fn, reads, writes, dma, key):
        self.eng, self.fn, self.reads, self.writes = eng, fn, reads, writes
        self.dma, self.key = dma, key
        self.deps = []
        self.need_inc = False
        self.cnt = 0
        self.sem = None


_BAR = {}


def init_barrier(nc, top):
    _BAR[id(nc)] = {"done": top.enter_context(nc.semaphore("bar_done")),
                    "clr": top.enter_context(nc.semaphore("bar_clr")), "n": 0}


def final_cleanup(nc):
    bar = _BAR[id(nc)]
    n = bar["n"]
    with nc.semaphore("bar_fin") as fin:
        with nc.Block() as block:
            def body(eng, is_sp):
                eng.sem_inc(fin, 1)
                if is_sp:
                    eng.wait_ge(fin, 5)
                    eng.sem_clear(bar["done"])
                    eng.sem_clear(bar["clr"])
                    eng.sem_clear(fin)

            @block.tensor
            def _(e):
                body(e, False)

            @block.scalar
            def _(e):
                body(e, False)

            @block.vector
            def _(e):
                body(e, False)

            @block.gpsimd
            def _(e):
                body(e, False)

            @block.sync
            def _(e):
                body(e, True)


class Phase:
    def __init__(self, nc, name="ph", same_engine_sync=True):
        self.nc = nc
        self.name = name
        self.ops = []
        self.stack = contextlib.ExitStack()
        self.same_engine_sync = same_engine_sync

    def sb(self, name, shape, dt):
        return self.stack.enter_context(self.nc.sbuf_tensor(f"{self.name}_{name}", list(shape), dt))

    def ps(self, name, shape, dt):
        return self.stack.enter_context(self.nc.psum_tensor(f"{self.name}_{name}", list(shape), dt))

    def op(self, eng, fn, reads=(), writes=(), dma=False, key=None):
        o = _Op(eng, fn, tuple(reads), tuple(writes), dma, key)
        o.idx = len(self.ops)
        self.ops.append(o)
        return o

    def dma(self, eng, fn, reads=(), writes=(), key=None):
        assert key is not None
        return self.op(eng, fn, reads, writes, dma=True, key=key)

    def close(self):
        nc = self.nc
        ops = self.ops
        last_w = {}
        readers = {}
        for o in ops:
            deps = set()
            for t in o.reads:
                w = last_w.get(t)
                if w is not None:
                    deps.add(w)
            for t in o.writes:
                w = last_w.get(t)
                if w is not None:
                    deps.add(w)
                for r in readers.get(t, ()):
                    deps.add(r)
            for t in o.reads:
                readers.setdefault(t, []).append(o)
            for t in o.writes:
                last_w[t] = o
                readers[t] = []
            deps.discard(o)
            dl = []
            for d in deps:
                if (not d.dma) and d.eng == o.eng and (o.eng == "pe" or not self.same_engine_sync):
                    continue
                dl.append(d)
                d.need_inc = True
            o.deps = dl
        sems = {}

        def getsem(k):
            if k not in sems:
                sems[k] = self.stack.enter_context(nc.semaphore(f"{self.name}_{k}"))
            return sems[k]

        cnts = {}
        for o in ops:
            if o.dma:
                k = ("d", o.key)
                o.sem = k
                cnts[k] = cnts.get(k, 0) + 16
                o.cnt = cnts[k]
            elif o.need_inc:
                k = ("e", o.eng)
                o.sem = k
                cnts[k] = cnts.get(k, 0) + 1
                o.cnt = cnts[k]
        for k in cnts:
            getsem(str(k[0]) + "_" + str(k[1]))
        final = dict(cnts)
        swd = set()
        for o in ops:
            if o.dma and o.eng == "pool":
                swd.add(str(o.sem[0]) + "_" + str(o.sem[1]))
            if o.dma:
                assert (o.eng == "pool") == ((str(o.sem[0]) + "_" + str(o.sem[1])) in swd), ("mixed SW/HW DGE on one key", o.key)
        per_eng = {e: [o for o in ops if o.eng == e] for e in ENGS}

        def emit(engname, eng):
            waited = {}
            for o in per_eng[engname]:
                need = {}
                for d in o.deps:
                    if need.get(d.sem, 0) < d.cnt:
                        need[d.sem] = d.cnt
                for k, v in need.items():
                    if waited.get(k, 0) >= v:
                        continue
                    eng.wait_ge(getsem(str(k[0]) + "_" + str(k[1])), v)
                    waited[k] = v
                ins = o.fn(eng)
                if o.dma:
                    ins.then_inc(getsem(str(o.sem[0]) + "_" + str(o.sem[1])), 16)
                elif o.need_inc:
                    ins.then_inc(getsem(str(o.sem[0]) + "_" + str(o.sem[1])), 1)
            for k, v in final.items():
                if waited.get(k, 0) >= v:
                    continue
                eng.wait_ge(getsem(str(k[0]) + "_" + str(k[1])), v)
            eng.sem_inc(bar["done"], 1)
            if engname == "pool":
                eng.wait_ge(bar["done"], 5 * (kph + 1))
                nums = sorted(sems[sn].num for sn in swd)
                i = 0
                while i < len(nums):
                    j = i
                    while j + 1 < len(nums) and nums[j + 1] == nums[j] + 1:
                        j += 1
                    eng.dma_reset(range(nums[i], nums[j] + 1))
                    i = j + 1
                for sname in sorted(sems):
                    eng.sem_clear(sems[sname])
                eng.sem_inc(bar["clr"], 1)
            eng.wait_ge(bar["clr"], kph + 1)

        bar = _BAR[id(nc)]
        kph = bar["n"]
        bar["n"] += 1
        with nc.Block() as block:
            @block.tensor
            def _(e):
                emit("pe", e)

            @block.scalar
            def _(e):
                emit("act", e)

            @block.vector
            def _(e):
                emit("dve", e)

            @block.gpsimd
            def _(e):
                emit("pool", e)

            @block.sync
            def _(e):
                emit("sp", e)
        self.stack.close()
        self.ops = []


D = 1024
S = 2048
NSEQ = 2
TOK = NSEQ * S
PW = 3080
NEXP = 32
CAP = 640
NEG = -30000.0
ALPHA = 2.0 ** 0.25
LN_EPS = 1e-5
RMS_EPS = 1e-6


class Consts:
    pass


def setup_consts(nc, top, cdram):
    init_barrier(nc, top)
    c = Consts()
    c.ident_bf = top.enter_context(nc.sbuf_tensor("ident_bf", [128, 128], BF16))
    c.ident_f = top.enter_context(nc.sbuf_tensor("ident_f", [128, 128], F32))
    c.maskb = top.enter_context(nc.sbuf_tensor("maskb", [128, 256], BF16))
    c.maskA = top.enter_context(nc.sbuf_tensor("maskA", [128, 512], BF16))
    c.maskB = top.enter_context(nc.sbuf_tensor("maskB", [128, 512], BF16))
    c.ones_f = top.enter_context(nc.sbuf_tensor("ones_f", [128, 128], F32))
    c.ones_bf = top.enter_context(nc.sbuf_tensor("ones_bf", [128, 128], BF16))
    c.tri_bf = top.enter_context(nc.sbuf_tensor("tri_bf", [128, 128], BF16))
    c.tris_bf = top.enter_context(nc.sbuf_tensor("tris_bf", [128, 128], BF16))
    c.tri_f = top.enter_context(nc.sbuf_tensor("tri_f", [128, 128], F32))
    c.tri4_bf = top.enter_context(nc.sbuf_tensor("tri4_bf", [128, 512], BF16))
    ph = Phase(nc, "cst")
    ph.dma("sp", lambda e: e.dma_start(out=c.ident_f[:], in_=cdram["ident"]), writes=["a"], key="a")
    ph.dma("sp", lambda e: e.dma_start(out=c.ones_f[:], in_=cdram["ones"]), writes=["b"], key="b")
    ph.dma("sp", lambda e: e.dma_start(out=c.tri_f[:], in_=cdram["tri"]), writes=["c"], key="c")
    ph.dma("pool", lambda e: e.dma_start(out=c.ident_bf[:], in_=cdram["ident"]), writes=["d"], key="d")
    ph.dma("pool", lambda e: e.dma_start(out=c.maskb[:], in_=cdram["maskb"]), writes=["e"], key="e")
    ph.dma("pool", lambda e: e.dma_start(out=c.maskA[:], in_=cdram["maskA"]), writes=["e01"], key="e01")
    ph.dma("pool", lambda e: e.dma_start(out=c.maskB[:], in_=cdram["maskB"]), writes=["e02"], key="e02")
    ph.dma("pool", lambda e: e.dma_start(out=c.ones_bf[:], in_=cdram["ones"]), writes=["f"], key="f")
    ph.dma("pool", lambda e: e.dma_start(out=c.tri_bf[:], in_=cdram["tri"]), writes=["g"], key="g")
    ph.dma("pool", lambda e: e.dma_start(out=c.tris_bf[:], in_=cdram["tris"]), writes=["h"], key="h")
    ph.dma("pool", lambda e: e.dma_start(out=c.tri4_bf[:], in_=cdram["tri4"]), writes=["i4"], key="i4")
    ph.close()
    return c


def host_consts():
    k = np.arange(128)[:, None]
    q = np.arange(128)[None, :]
    maskb = np.zeros((128, 256), np.float32)
    maskb[:, 0:128] = np.where(k <= q, 0.0, NEG)
    maskb[:, 128:256] = np.where(k >= q, 0.0, NEG)
    return {
        "c_ident": np.eye(128, dtype=np.float32),
        "c_ones": np.ones((128, 128), np.float32),
        "c_tri": (k <= q).astype(np.float32),
        "c_tris": (k < q).astype(np.float32),
        "c_tri4": np.tile((k <= q).astype(np.float32), (1, 4)),
        "c_maskb": maskb,
        "c_maskA": np.tile((maskb == 0.0).astype(np.float32), (1, 2)),
        "c_maskB": np.tile((maskb[:, 0:128] == 0.0).astype(np.float32), (1, 4)),
    }


def attention_phase(nc, c, s, xT_dram, w_in, xT, attnT, dbg=None, Xg=None):
    ph = Phase(nc, f"at{s}")
    wqkv = ph.sb("wqkv", [128, 8, 1536], BF16)
    w_in_v = w_in.rearrange("(c p) n -> p c n", p=128)
    xT_v = xT_dram[s].rearrange("(c p) t -> p c t", p=128)

    def ld(cc):
        ph.dma("pool", lambda e: e.dma_start(out=xT[:, cc, :], in_=xT_v[:, cc, :]),
               writes=[("xT", cc)], key=f"xT{cc}")
        ph.dma("pool", lambda e: e.dma_start(out=wqkv[:, cc, :], in_=w_in_v[:, cc, 0:1536]),
               writes=[("wqkv", cc)], key=f"wq{cc}")
    for cc in range(8):
        ld(cc)
    if Xg is not None:
        zt = ph.sb("zt", [128, 8192], BF16)
        ph.op("dve", lambda e: e.memset(zt[:], 0.0), writes=["zt"])
        Xg_v = Xg.rearrange("(p r) d -> p (r d)", p=128)
        for i in range(NROWS // 128 * D // 8192):
            (lambda i: ph.dma("sp", lambda e: e.dma_start(out=Xg_v[:, 8192 * i: 8192 * (i + 1)], in_=zt[:]), reads=["zt"], key="zx"))(i)
    QT = [ph.sb(f"QT{i}", [128, S], BF16) for i in range(2)]
    KT = [ph.sb(f"KT{i}", [128, S], BF16) for i in range(2)]
    V3 = [[ph.sb(f"V{i}_{l}", [128, 16, 2, 65], BF16) for l in range(3)] for i in range(2)]

    def ms(t, tok):
        ph.op("pool", lambda e: e.memset(t[:], 1.0), writes=[tok])
    for i in range(2):
        for l in range(3):
            ms(V3[i][l], ("V", i, l))
    pj = [ph.ps(f"pj{i}", [128, 512], F32) for i in range(2)]
    st = [ph.ps(f"st{i}", [128, 512], F32) for i in range(2)]
    acc = ph.ps("acc", [128, S], F32)
    pt = [ph.sb(f"pt{i}", [128, 512], BF16) for i in range(4)]
    pe = [ph.sb(f"pe{i}", [128, 512], BF16) for i in range(4)]
    rc = ph.sb("rc", [128, S], F32)
    bcs = [ph.sb(f"bcs{i}", [64, 512], F32) for i in range(2)]
    cnt = {"pj": 0, "st": 0, "pt": 0, "ev": 0}

    def evac(out, in_, reads, writes):
        cnt["ev"] += 1
        if cnt["ev"] % 3 == 0:
            ph.op("act", lambda e: e.copy(out=out, in_=in_), reads=reads, writes=writes)
        else:
            ph.op("dve", lambda e: e.tensor_copy(out=out, in_=in_), reads=reads, writes=writes)

    def mm(out, lhsT, rhs, start, stop, reads, writes, skip=False):
        ph.op("pe", lambda e: e.matmul(out, lhsT=lhsT, rhs=rhs, start=start, stop=stop, skip_group_check=skip),
              reads=reads, writes=writes)

    def nextpj():
        i = cnt["pj"] % 2
        cnt["pj"] += 1
        return pj[i], ("pj", i)

    sbufs = [(st[0], ("st", 0)), (st[1], ("st", 1)), (pj[0], ("pj", 0)), (pj[1], ("pj", 1))]

    def score_group(bi, pr, blocks, mask):
        sb_, stok = sbufs[cnt["st"] % 4]
        cnt["st"] += 1
        qk_reads = [("QT", bi, n) for n in range(4)] + [("KT", bi, n) for n in range(4)]
        off = 0
        offs = []
        for (ksl, qsl, nq, pvs) in blocks:
            mm(sb_[:, off:off + nq], KT[bi][pr, ksl], QT[bi][pr, qsl], True, True, qk_reads, [stok])
            offs.append(off)
            off += nq
        tot = off
        pi = cnt["pt"] % 4
        cnt["pt"] += 1
        ph.op("act", lambda e: e.activation(out=pe[pi][:, 0:tot], in_=sb_[:, 0:tot], func=AF.Exp, scale=0.125),
              reads=[stok], writes=[("pe", pi)])
        ph.op("dve", lambda e: e.tensor_tensor(out=pt[pi][:, 0:tot], in0=pe[pi][:, 0:tot], in1=mask[:, 0:tot], op=ALU.mult),
              reads=[("pe", pi)], writes=[("pt", pi)])
        return pi, offs

    def norm(h):
        for b in range(4):
            (lambda bsl: ph.op("act", lambda e: e.activation(out=rc[64:65, bsl], in_=acc[64:65, bsl], func=AF.Ln),
                               reads=[("acc", b)], writes=[("rc", b)]))(slice(512 * b, 512 * b + 512))
            (lambda bsl: ph.op("act", lambda e: e.activation(out=rc[64:65, bsl], in_=rc[64:65, bsl], func=AF.Exp, scale=-1.0),
                               reads=[("rc", b)], writes=[("rc", b)]))(slice(512 * b, 512 * b + 512))
        for b in range(4):
            bsl = slice(512 * b, 512 * b + 512)
            p, ptok = nextpj()
            bi2 = cnt["ev"] % 2
            cnt["ev"] += 1
            mm(p[0:64, :], c.ones_f[64:65, 0:64], rc[64:65, bsl], True, True, [("rc", b)], [ptok])
            (lambda p, bi2: ph.op("act", lambda e: e.copy(out=bcs[bi2][:, :], in_=p[0:64, :]), reads=[ptok], writes=[("bcs", bi2)]))(p, bi2)
            (lambda bsl, bi2: ph.op("dve", lambda e: e.tensor_tensor(out=attnT[:, h, bsl], in0=acc[0:64, bsl], in1=bcs[bi2][:, :], op=ALU.mult),
                                    reads=[("bcs", bi2), ("acc", b)], writes=[("attnT", h, b), ("acc", b)]))(bsl, bi2)

    for a in range(4):
        bi = a % 2
        for (dst, col0, nm) in ((QT[bi], 0, "QT"), (KT[bi], 512, "KT")):
            for n in range(4):
                p, ptok = nextpj()
                for cc in range(8):
                    mm(p[:, :], wqkv[:, cc, col0 + 128 * a: col0 + 128 * a + 128], xT[:, cc, 512 * n: 512 * n + 512],
                       cc == 0, cc == 7, [("xT", cc), ("wqkv", cc)], [ptok])
                evac(dst[:, 512 * n: 512 * n + 512], p[:, :], [ptok], [(nm, bi, n)])
        for l, dil in enumerate((1, 4, 16)):
            for g0 in range(0, 16, 4):
                p, ptok = nextpj()
                for gi in range(4):
                    g = g0 + gi
                    if dil == 1:
                        tsl = slice(128 * g, 128 * g + 128)
                    elif dil == 4:
                        r, n = g // 4, g % 4
                        tsl = slice(512 * n + r, 512 * n + r + 509, 4)
                    else:
                        tsl = slice(g, g + 2033, 16)
                    for cc in range(8):
                        mm(p[:, 128 * gi: 128 * gi + 128], xT[:, cc, tsl],
                           wqkv[:, cc, 1024 + 128 * a: 1024 + 128 * a + 128], cc == 0, cc == 7,
                           [("xT", cc), ("wqkv", cc)], [ptok])
                evac(V3[bi][l][:, g0:g0 + 4, :, 0:64],
                     p[:, :].rearrange("p (g j e) -> p g j e", g=4, j=2), [ptok], [("V", bi, l)])
        tasks = []
        for j in range(2):
            h = 2 * a + j
            pr = slice(64 * j, 64 * j + 64)
            b1 = []
            for n in range(16):
                nq = 256 if n < 15 else 128
                pvs = [(slice(128 * (qb - n), 128 * (qb - n) + 128), V3[bi][0][:, n, j, :], slice(128 * qb, 128 * qb + 128), qb // 4)
                       for qb in range(n, min(n + 2, 16))]
                b1.append((slice(128 * n, 128 * n + 128), slice(128 * n, 128 * n + nq), nq, pvs))
            for g in range(0, 16, 2):
                tasks.append(("grp", h, pr, b1[g:g + 2], c.maskA))
            for r in range(4):
                b2 = []
                for n in range(4):
                    nq = 256 if n < 3 else 128
                    b0 = 512 * n + r
                    pvs = [(slice(128 * (qb - n), 128 * (qb - n) + 128), V3[bi][1][:, 4 * r + n, j, :],
                            slice(512 * qb + r, 512 * qb + r + 509, 4), qb) for qb in range(n, min(n + 2, 4))]
                    b2.append((slice(b0, b0 + 509, 4), slice(b0, b0 + 4 * nq - 3, 4), nq, pvs))
                tasks.append(("grp", h, pr, b2[0:2], c.maskA))
                tasks.append(("grp", h, pr, b2[2:4], c.maskA))
            b3 = []
            for r in range(16):
                pvs = [(slice(32 * b, 32 * b + 32), V3[bi][2][:, r, j, :], slice(512 * b + r, 512 * b + r + 497, 16), b) for b in range(4)]
                b3.append((slice(r, r + 2033, 16), slice(r, r + 2033, 16), 128, pvs))
            for g in range(0, 16, 4):
                tasks.append(("grp", h, pr, b3[g:g + 4], c.maskB))
            tasks.append(("norm", h))
        started = {}
        vreads = [("V", bi, 0), ("V", bi, 1), ("V", bi, 2)]

        def emit_pv(t, pi, offs):
            h = t[1]
            for (blk, off) in zip(t[3], offs):
                for (psl, vap, osl, bank) in blk[3]:
                    first = not started.get((h, bank), False)
                    started[(h, bank)] = True
                    mm(acc[0:65, osl], vap, pt[pi][:, off + psl.start: off + psl.stop], first, True, [("pt", pi)] + vreads,
                       [("acc", bank)], skip=True)

        pending = []
        for t in tasks:
            if t[0] == "grp":
                pi, offs = score_group(bi, t[2], t[3], t[4])
                pending.append((t, pi, offs))
                if len(pending) > 2:
                    emit_pv(*pending.pop(0))
            else:
                while pending:
                    emit_pv(*pending.pop(0))
                norm(t[1])
    if dbg is not None:
        ph.dma("sp", lambda e: e.dma_start(out=dbg, in_=attnT[:]),
               reads=[("attnT", h, b) for h in range(8) for b in range(4)], key="dbg")
    ph.close()


def host_small(inputs):
    conv_w = inputs["conv_w"][0]
    conv_b = inputs["conv_b"][0]
    gb = np.concatenate([inputs["b_igate"][0], inputs["b_fgate"][0]])
    rep = lambda v: np.ascontiguousarray(np.broadcast_to(v[None, :], (128, v.shape[0]))).astype(np.float32)
    return {
        "s_cwT": np.ascontiguousarray(conv_w.reshape(4, 4, 128).transpose(2, 1, 0)).reshape(128, 16),
        "s_cbT": np.ascontiguousarray(conv_b.reshape(4, 128).T),
        "s_gb": rep(np.tile(gb, 16)),
        "s_mgT": np.ascontiguousarray(inputs["mnorm_g"][0].reshape(4, 128).T),
        "s_ln1g": rep(inputs["ln1_g"][0]), "s_ln1b": rep(inputs["ln1_b"][0]),
        "s_ln2g": rep(inputs["ln2_g"][0]), "s_ln2b": rep(inputs["ln2_b"][0]),
        "s_brt": rep(inputs["b_router"][0]),
        "s_ecap": rep(np.arange(NEXP, dtype=np.float32) * CAP),
    }


def mlstm_phase(nc, c, s, w_in, wmq_d, wmk_d, sm, xT, hmT, dbg=None):
    ph = Phase(nc, f"ml{s}")
    w_in_v = w_in.rearrange("(c p) n -> p c n", p=128)
    wm = ph.sb("wm", [128, 8, 1544], BF16)

    def ldw(cc):
        ph.dma("pool", lambda e: e.dma_start(out=wm[:, cc, :], in_=w_in_v[:, cc, 1536:3080]),
               writes=[("wm", cc)], key=f"wm{cc}")
    for cc in range(8):
        ldw(cc)
    wq = ph.sb("wq", [128, 4, 128], BF16)
    wk = ph.sb("wk", [128, 4, 128], BF16)
    ph.dma("pool", lambda e: e.dma_start(out=wq[:], in_=wmq_d.rearrange("h e f -> e h f")), writes=["wq"], key="wq")
    ph.dma("pool", lambda e: e.dma_start(out=wk[:], in_=wmk_d.rearrange("h e f -> e h f")), writes=["wk"], key="wk")
    cw = ph.sb("cw", [128, 16], F32)
    cb = ph.sb("cb", [128, 4], F32)
    gb = ph.sb("gb", [128, 128], F32)
    mgT = ph.sb("mgT", [128, 4], F32)
    ph.dma("sp", lambda e: e.dma_start(out=cw[:], in_=sm["cwT"]), writes=["cw"], key="cw")
    ph.dma("sp", lambda e: e.dma_start(out=cb[:], in_=sm["cbT"]), writes=["cb"], key="cb")
    ph.dma("sp", lambda e: e.dma_start(out=gb[:], in_=sm["gb"]), writes=["gb"], key="gb")
    ph.dma("sp", lambda e: e.dma_start(out=mgT[:], in_=sm["mgT"]), writes=["mgT"], key="mgT")
    xall = [("xT", cc) for cc in range(8)]

    pj = [ph.ps(f"pj{i}", [128, 512], F32) for i in range(2)]
    misc = ph.ps("misc", [128, 512], F32)
    st = [ph.ps(f"st{i}", [128, 512], F32) for i in range(2)]
    ob = [ph.ps(f"ob{i}", [128, 512], F32) for i in range(2)]
    tpb = ph.ps("tpb", [128, 512], F32)
    cnt = {"pj": 0, "ev": 0, "st": 0, "ob": 0}

    def mm(out, lhsT, rhs, start, stop, reads, writes):
        ph.op("pe", lambda e: e.matmul(out, lhsT=lhsT, rhs=rhs, start=start, stop=stop), reads=reads, writes=writes)

    def nextpj():
        i = cnt["pj"] % 2
        cnt["pj"] += 1
        return pj[i], ("pj", i)

    def evac(out, in_, reads, writes):
        cnt["ev"] += 1
        if cnt["ev"] % 2 == 0:
            ph.op("act", lambda e: e.copy(out=out, in_=in_), reads=reads, writes=writes)
        else:
            ph.op("dve", lambda e: e.tensor_copy(out=out, in_=in_), reads=reads, writes=writes)

    def dve(fn, reads, writes):
        ph.op("dve", fn, reads=reads, writes=writes)

    def act(fn, reads, writes):
        ph.op("act", fn, reads=reads, writes=writes)

    for ck in range(16):
        for cc in range(8):
            mm(misc[:, 8 * ck: 8 * ck + 8], xT[:, cc, 128 * ck: 128 * ck + 128], wm[:, cc, 1536:1544],
               cc == 0, cc == 7, [("xT", cc), ("wm", cc)], ["misc"])
    gsb = ph.sb("gsb", [128, 128], F32)
    dve(lambda e: e.tensor_tensor(out=gsb[:], in0=misc[:, 0:128], in1=gb[:], op=ALU.add), ["misc", "gb"], ["gsb"])
    gsb3 = gsb[:, :].rearrange("p (c g) -> p c g", g=8)
    sp = ph.sb("sp", [128, 16, 4], F32)
    eb = ph.sb("eb", [128, 16, 4], F32)
    eg = ph.sb("eg", [128, 16, 4], F32)
    egl = ph.sb("egl", [128, 16, 4], F32)
    act(lambda e: e.activation(out=sp[:], in_=gsb3[:, :, 4:8], func=AF.Exp, scale=-1.0), ["gsb"], ["sp"])
    act(lambda e: e.activation(out=sp[:], in_=sp[:], func=AF.Ln, bias=1.0), ["sp"], ["sp"])
    spf = sp[:, :, :].rearrange("p c g -> p (c g)")
    mm(misc[:, 128:192], c.tri_f[:, :], spf, True, True, ["sp"], ["misc"])
    mm(misc[:, 192:256], c.ones_f[:, :], spf, True, True, ["sp"], ["misc"])
    ebf = eb[:, :, :].rearrange("p c g -> p (c g)")
    dve(lambda e: e.tensor_tensor(out=eb[:], in0=misc[:, 128:192].rearrange("p (c g) -> p c g", g=4),
                                  in1=gsb3[:, :, 0:4], op=ALU.add), ["misc", "gsb"], ["eb"])
    act(lambda e: e.activation(out=ebf, in_=ebf, func=AF.Exp), ["eb"], ["eb"])
    act(lambda e: e.activation(out=eg[:, :, :].rearrange("p c g -> p (c g)"), in_=misc[:, 128:192], func=AF.Exp,
                               scale=-1.0), ["misc"], ["eg"])
    act(lambda e: e.activation(out=egl[:, :, :].rearrange("p c g -> p (c g)"), in_=misc[:, 192:256], func=AF.Exp,
                               scale=-1.0), ["misc"], ["egl"])

    xmpad = [ph.sb("xmpad0", [128, S + 3], F32)] * 2
    ctmp = [ph.sb("ctmp0", [128, S], F32)] * 2
    xc = [ph.sb(f"xc{i}", [128, S], BF16) for i in range(2)]
    qT = [ph.sb(f"qT{i}", [128, S], BF16) for i in range(2)]
    kT = [ph.sb(f"kT{i}", [128, S], BF16) for i in range(2)]
    ktok = [ph.sb(f"ktok{i}", [128, 16, 128], BF16) for i in range(2)]
    Vs = [ph.sb(f"Vs{i}", [128, 16, 129], BF16) for i in range(2)]
    sgo = [ph.sb(f"sgo{i}", [128, 16, 128], BF16) for i in range(2)]
    Call = [ph.sb("Call0", [128, 16, 129], F32)] * 2
    Cball = [ph.sb(f"Cball{i}", [128, 16, 129], BF16) for i in range(2)]
    pT = [ph.sb("pT0", [128, S], BF16)] * 2
    hm3 = [ph.sb(f"hm3{i}", [128, 3, 128], F32) for i in range(2)]
    junk = [ph.sb("junk0", [128, 128], F32)] * 2
    sm1 = [ph.sb(f"sm1{i}", [128, 20], F32) for i in range(2)]
    ph.op("pool", lambda e: e.memset(xmpad[0][:, 0:3], 0.0), writes=[("xmp", 0)])

    def inproj(h):
        bi = h % 2
        for n in range(4):
            p, ptok = nextpj()
            for cc in range(8):
                mm(p[:, :], wm[:, cc, 128 * h: 128 * h + 128], xT[:, cc, 512 * n: 512 * n + 512], cc == 0, cc == 7,
                   [("xT", cc), ("wm", cc)], [ptok])
            evac(xmpad[bi][:, 3 + 512 * n: 3 + 512 * n + 512], p[:, :], [ptok, ("xmp", 0)], [("xm", 0, n)])
        xmr = [("xm", 0, n) for n in range(4)]
        dve(lambda e: e.tensor_scalar(out=ctmp[bi][:, :], in0=xmpad[bi][:, 0:S], scalar1=cw[:, 4 * h: 4 * h + 1],
                                      scalar2=cb[:, h: h + 1], op0=ALU.mult, op1=ALU.add),
            xmr + ["cw", "cb"], [("ctmp", 0)])
        for j in range(1, 4):
            (lambda j: dve(lambda e: e.scalar_tensor_tensor(out=ctmp[bi][:, :], in0=xmpad[bi][:, j: S + j],
                                                            scalar=cw[:, 4 * h + j: 4 * h + j + 1], in1=ctmp[bi][:, :],
                                                            op0=ALU.mult, op1=ALU.add),
                           xmr + [("ctmp", 0)], [("ctmp", 0)]))(j)
        for c4 in range(4):
            p, ptok = nextpj()
            for i in range(4):
                ck = 4 * c4 + i
                for cc in range(8):
                    mm(p[:, 128 * i: 128 * i + 128], xT[:, cc, 128 * ck: 128 * ck + 128],
                       wm[:, cc, 512 + 128 * h: 512 + 128 * h + 128], cc == 0, cc == 7, [("xT", cc), ("wm", cc)], [ptok])
            for i in range(4):
                ck = 4 * c4 + i
                (lambda p, i, ck: act(lambda e: e.activation(out=Vs[bi][:, ck, 0:128], in_=p[:, 128 * i: 128 * i + 128], func=AF.Copy,
                                                            scale=eb[:, ck, h: h + 1]), [ptok, "eb"], [("Vs", bi)]))(p, i, ck)
        ph.op("pool", lambda e: e.tensor_copy(out=Vs[bi][:, :, 128], in_=eb[:, :, h]), reads=["eb"], writes=[("Vs", bi)])
        for c4 in range(4):
            p, ptok = nextpj()
            for i in range(4):
                ck = 4 * c4 + i
                for cc in range(8):
                    mm(p[:, 128 * i: 128 * i + 128], xT[:, cc, 128 * ck: 128 * ck + 128],
                       wm[:, cc, 1024 + 128 * h: 1024 + 128 * h + 128], cc == 0, cc == 7, [("xT", cc), ("wm", cc)], [ptok])
            (lambda p, c4: act(lambda e: e.activation(out=sgo[bi][:, 4 * c4: 4 * c4 + 4, :],
                                                      in_=p[:, :].rearrange("p (i f) -> p i f", i=4), func=AF.Sigmoid),
                               [ptok], [("sgo", bi)]))(p, c4)
        act(lambda e: e.activation(out=xc[bi][:, :], in_=ctmp[bi][:, :], func=AF.Silu), [("ctmp", 0)], [("xc", bi)])
        for n in range(4):
            p, ptok = nextpj()
            mm(p[:, :], wq[:, h, :], xc[bi][:, 512 * n: 512 * n + 512], True, True, [("xc", bi), "wq"], [ptok])
            (lambda p, n: act(lambda e: e.activation(out=qT[bi][:, 512 * n: 512 * n + 512], in_=p[:, :], func=AF.Copy,
                                                     scale=float(128 ** -0.5)), [ptok], [("qT", bi)]))(p, n)
            p, ptok = nextpj()
            mm(p[:, :], wk[:, h, :], xc[bi][:, 512 * n: 512 * n + 512], True, True, [("xc", bi), "wk"], [ptok])
            evac(kT[bi][:, 512 * n: 512 * n + 512], p[:, :], [ptok], [("kT", bi)])
        for c4 in range(4):
            p, ptok = nextpj()
            for i in range(4):
                ck = 4 * c4 + i
                mm(p[:, 128 * i: 128 * i + 128], xc[bi][:, 128 * ck: 128 * ck + 128], wk[:, h, :], True, True,
                   [("xc", bi), "wk"], [ptok])
            evac(ktok[bi][:, 4 * c4: 4 * c4 + 4, :], p[:, :].rearrange("p (i f) -> p i f", i=4), [ptok], [("ktok", bi)])

    GROUPS = [(0, 3), (3, 3), (6, 3), (9, 3), (12, 3), (15, 1)]

    def stageA(h):
        bi = h % 2
        dve(lambda e: e.memset(Call[bi][:, 0, :], 0.0), [], [("Call", 0, 0)])
        for (c0, n) in GROUPS[:5]:
            p, ptok = nextpj()
            for i in range(n):
                ck = c0 + i
                mm(p[:, 129 * i: 129 * i + 129], ktok[bi][:, ck, :], Vs[bi][:, ck, :], True, True, [("ktok", bi), ("Vs", bi)], [ptok])
            for i in range(n):
                ck = c0 + i
                (lambda p, i, ck: act(lambda e: e.activation(out=Call[bi][:, ck + 1, :], in_=p[:, 129 * i: 129 * i + 129], func=AF.Copy,
                                                             scale=egl[:, ck, h: h + 1]), [ptok, "egl"], [("Call", 0, ck + 1)]))(p, i, ck)
        for ck in range(1, 15):
            (lambda ck: dve(lambda e: e.scalar_tensor_tensor(out=Call[bi][:, ck + 1, :], in0=Call[bi][:, ck, :], scalar=egl[:, ck, h: h + 1],
                                                             in1=Call[bi][:, ck + 1, :], op0=ALU.mult, op1=ALU.add),
                            [("Call", 0, ck), ("Call", 0, ck + 1), "egl"], [("Call", 0, ck + 1)]))(ck)
        act(lambda e: e.copy(out=Cball[bi][:, :, :], in_=Call[bi][:, :, :]), [("Call", 0, ck) for ck in range(16)], [("Cball", bi)])

    def stageB(h):
        bi = h % 2
        for q4 in range(4):
            si = cnt["st"] % 2
            cnt["st"] += 1
            for i in range(4):
                ck = 4 * q4 + i
                csl = slice(128 * ck, 128 * ck + 128)
                mm(st[si][:, 128 * i: 128 * i + 128], kT[bi][:, csl], qT[bi][:, csl], True, True, [("kT", bi), ("qT", bi)], [("st", si)])
            (lambda si, q4: dve(lambda e: e.tensor_tensor(out=pT[bi][:, 512 * q4: 512 * q4 + 512], in0=st[si][:, :], in1=c.tri4_bf[:, :], op=ALU.mult),
                                [("st", si)], [("pT", 0, q4)]))(si, q4)
        for (c0, n) in GROUPS:
            oi = cnt["ob"] % 2
            cnt["ob"] += 1
            o = ob[oi]
            otok = ("ob", oi)
            for i in range(n):
                ck = c0 + i
                csl = slice(128 * ck, 128 * ck + 128)
                mm(o[:, 129 * i: 129 * i + 129], pT[bi][:, csl], Vs[bi][:, ck, :], True, ck == 0, [("pT", 0, ck // 4), ("Vs", bi)], [otok])
                if ck > 0:
                    mm(o[:, 129 * i: 129 * i + 129], qT[bi][:, csl], Cball[bi][:, ck, :], False, True, [("qT", bi), ("Cball", bi)], [otok])
            o3 = o[:, 0: 129 * n].rearrange("p (i f) -> p i f", f=129)
            s1 = sm1[oi]
            eg3 = eg[:, c0: c0 + n, h]

            def grp(o, o3, s1, eg3, c0, n, oi, otok):
                dve(lambda e: e.tensor_tensor(out=s1[:, 0:n], in0=o3[:, :, 128], in1=eg3, op=ALU.mult), [otok, "eg"], [("s1", oi)])
                act(lambda e: e.activation(out=s1[:, 0:n], in_=s1[:, 0:n], func=AF.Abs), [("s1", oi)], [("s1", oi)])
                dve(lambda e: e.tensor_scalar(out=s1[:, 0:n], in0=s1[:, 0:n], scalar1=1.0, scalar2=None, op0=ALU.max), [("s1", oi)], [("s1", oi)])
                dve(lambda e: e.reciprocal(out=s1[:, 0:n], in_=s1[:, 0:n]), [("s1", oi)], [("s1", oi)])
                dve(lambda e: e.tensor_tensor(out=s1[:, 4:4 + n], in0=s1[:, 0:n], in1=eg3, op=ALU.mult), [("s1", oi), "eg"], [("fac", oi)])
                for i in range(n):
                    (lambda i: act(lambda e: e.activation(out=junk[oi][:, :], in_=o[:, 129 * i: 129 * i + 128], func=AF.Square,
                                                          scale=s1[:, 4 + i: 5 + i], accum_out=s1[:, 8 + i: 9 + i]),
                                   [otok, ("fac", oi)], [("ss", oi, i), ("junk", 0)]))(i)
                dve(lambda e: e.tensor_scalar(out=s1[:, 12:12 + n], in0=s1[:, 8:8 + n], scalar1=1.0 / 128.0, scalar2=RMS_EPS,
                                              op0=ALU.mult, op1=ALU.add), [("ss", oi, i) for i in range(n)], [("t3", oi)])
                act(lambda e: e.activation(out=s1[:, 12:12 + n], in_=s1[:, 12:12 + n], func=AF.Sqrt), [("t3", oi)], [("t3", oi)])
                dve(lambda e: e.reciprocal(out=s1[:, 12:12 + n], in_=s1[:, 12:12 + n]), [("t3", oi)], [("t3", oi)])
                dve(lambda e: e.tensor_tensor(out=s1[:, 16:16 + n], in0=s1[:, 12:12 + n], in1=s1[:, 4:4 + n], op=ALU.mult),
                    [("t3", oi), ("fac", oi)], [("sc2", oi)])
                for i in range(n):
                    (lambda i: dve(lambda e: e.scalar_tensor_tensor(out=hm3[oi][:, i, :], in0=o[:, 129 * i: 129 * i + 128], scalar=s1[:, 16 + i: 17 + i],
                                                                    in1=sgo[bi][:, c0 + i, :], op0=ALU.mult, op1=ALU.mult),
                                   [otok, ("sc2", oi), ("sgo", bi)], [("hm3", oi, i)]))(i)
                for i in range(n):
                    (lambda i: ph.op("pe", lambda e: e.transpose(out=tpb[:, 128 * i: 128 * i + 128], in_=hm3[oi][:, i, :], identity=c.ident_f[:, :]),
                                     reads=[("hm3", oi, i)], writes=["tpb"]))(i)
                act(lambda e: e.activation(out=hmT[:, h, 128 * c0: 128 * (c0 + n)], in_=tpb[:, 0: 128 * n], func=AF.Copy, scale=mgT[:, h: h + 1]),
                    ["tpb", "mgT"], [("hmT", h, c0)])
            grp(o, o3, s1, eg3, c0, n, oi, otok)

    inproj(0)
    stageA(0)
    inproj(1)
    stageA(1)
    stageB(0)
    inproj(2)
    stageA(2)
    stageB(1)
    inproj(3)
    stageA(3)
    stageB(2)
    stageB(3)
    if dbg is not None:
        ph.dma("sp", lambda e: e.dma_start(out=dbg, in_=hmT[:]),
               reads=[("hmT", h, c0) for h in range(4) for (c0, n) in GROUPS], key="dbg")
    ph.close()


def outproj_phase(nc, c, s, x_d, w_out, w_router, sm, attnT, hmT, P, h1d, Xg, dbg=None):
    ph = Phase(nc, f"op{s}")
    woA = ph.sb("woA", [64, 8, D], BF16)
    woM = ph.sb("woM", [128, 4, D], BF16)
    woA_v = w_out[0:512, :].rearrange("(h e) n -> e h n", e=64)
    woM_v = w_out[512:1024, :].rearrange("(h f) n -> f h n", f=128)

    def ldw(h):
        ph.dma("pool", lambda e: e.dma_start(out=woA[:, h, :], in_=woA_v[:, h, :]), writes=[("woA", h)], key=f"woA{h}")
        if h < 4:
            ph.dma("pool", lambda e: e.dma_start(out=woM[:, h, :], in_=woM_v[:, h, :]), writes=[("woM", h)], key=f"woM{h}")
    for h in range(8):
        ldw(h)
    lng = ph.sb("lng", [128, D], F32)
    lnb = ph.sb("lnb", [128, D], F32)
    brt = ph.sb("brt", [128, 32], F32)
    ecap = ph.sb("ecap", [128, 32], F32)
    wr = ph.sb("wr", [128, 8, 32], F32)
    ph.dma("sp", lambda e: e.dma_start(out=lng[:], in_=sm["ln1g"]), writes=["lng"], key="lng")
    ph.dma("sp", lambda e: e.dma_start(out=lnb[:], in_=sm["ln1b"]), writes=["lnb"], key="lnb")
    ph.dma("sp", lambda e: e.dma_start(out=brt[:], in_=sm["brt"]), writes=["brt"], key="brt")
    ph.dma("sp", lambda e: e.dma_start(out=ecap[:], in_=sm["ecap"]), writes=["ecap"], key="ecap")
    ph.dma("sp", lambda e: e.dma_start(out=wr[:], in_=w_router.rearrange("(c p) n -> p c n", p=128)), writes=["wr"], key="wr")
    if s == 0:
        ph.op("dve", lambda e: e.memset(P.base[:], 0.0), writes=["base"])
        if dbg is not None:
            ph.op("pool", lambda e: e.memset(P.rows[:], 0), writes=[("rows", t) for t in range(32)])
            ph.op("pool", lambda e: e.memset(P.gates[:], 0.0), writes=[("gates", t) for t in range(32)])
    woAr = [("woA", h) for h in range(8)]
    woMr = [("woM", h) for h in range(4)]

    yp = [ph.ps(f"yp{i}", [128, D], F32) for i in range(2)]
    tp = ph.ps("tp", [128, D], F32)
    lgp = ph.ps("lgp", [128, 32], F32)
    pfp = ph.ps("pfp", [128, 64], F32)
    xt = [ph.sb(f"xt{i}", [128, D], F32) for i in range(2)]
    r = [ph.sb(f"r{i}", [128, D], F32) for i in range(2)]
    h1 = [ph.sb(f"h1{i}", [128, D], F32) for i in range(2)]
    h1b = [ph.sb(f"h1b{i}", [128, D], BF16) for i in range(2)]
    h1T = [ph.sb(f"h1T{i}", [128, 8, 128], F32) for i in range(2)]
    st6 = [ph.sb(f"st6{i}", [128, 2, 6], F32) for i in range(2)]
    sv = [ph.sb(f"sv{i}", [128, 16], F32) for i in range(2)]
    lg = [ph.sb(f"lg{i}", [128, 32], F32) for i in range(2)]
    lgraw = [ph.sb(f"lgraw{i}", [128, 32], F32) for i in range(2)]
    t8 = [ph.sb(f"t8{i}", [128, 8], F32) for i in range(2)]
    e4 = [ph.sb(f"e4{i}", [128, 4], F32) for i in range(2)]
    Mf = [ph.sb(f"Mf{i}", [128, 32], F32) for i in range(2)]
    Mb = [ph.sb(f"Mb{i}", [128, 32], BF16) for i in range(2)]
    ex = [ph.sb(f"ex{i}", [128, 32], F32) for i in range(2)]
    rowf = [ph.sb(f"rowf{i}", [128, 32], F32) for i in range(2)]
    j32 = [ph.sb(f"j32{i}", [128, 32], F32) for i in range(2)]
    rk = [ph.sb(f"rk{i}", [128, 4], F32) for i in range(2)]

    def mm(out, lhsT, rhs, start, stop, reads, writes):
        ph.op("pe", lambda e: e.matmul(out, lhsT=lhsT, rhs=rhs, start=start, stop=stop), reads=reads, writes=writes)

    def dve(fn, reads, writes):
        ph.op("dve", fn, reads=reads, writes=writes)

    def act(fn, reads, writes):
        ph.op("act", fn, reads=reads, writes=writes)

    def pool(fn, reads, writes):
        ph.op("pool", fn, reads=reads, writes=writes)

    def stage1(tt):
        gt = 16 * s + tt
        b = tt % 2
        tsl = slice(128 * tt, 128 * tt + 128)
        ph.dma("sp", lambda e: e.dma_start(out=xt[b][:], in_=x_d[128 * gt: 128 * gt + 128, :]), writes=[("xt", b)], key=f"xt{b}")
        for nh in range(2):
            nsl = slice(512 * nh, 512 * nh + 512)
            for h in range(8):
                mm(yp[b][:, nsl], attnT[:, h, tsl], woA[:, h, nsl], h == 0, False, [("attnT", h), ("woA", h)], [("yp", b, nh)])
            for h in range(4):
                mm(yp[b][:, nsl], hmT[:, h, tsl], woM[:, h, nsl], False, h == 3, [("hmT", h), ("woM", h)], [("yp", b, nh)])

    def tile(tt):
        gt = 16 * s + tt
        b = tt % 2
        tsl = slice(128 * tt, 128 * tt + 128)
        for nh in range(2):
            nsl = slice(512 * nh, 512 * nh + 512)
            (lambda nsl, nh: dve(lambda e: e.scalar_tensor_tensor(out=r[b][:, nsl], in0=xt[b][:, nsl], scalar=float(ALPHA),
                                                                  in1=yp[b][:, nsl], op0=ALU.mult, op1=ALU.add),
                                 [("xt", b), ("yp", b, nh)], [("r", b, nh)]))(nsl, nh)
            (lambda nsl, nh: dve(lambda e: e.bn_stats(out=st6[b][:, nh, :], in_=r[b][:, nsl]), [("r", b, nh)], [("st6", b, nh)]))(nsl, nh)
        svb = sv[b]
        dve(lambda e: e.bn_aggr(out=svb[:, 0:2], in_=st6[b][:, :, :].rearrange("p a b -> p (a b)")),
            [("st6", b, 0), ("st6", b, 1)], [("sv", b)])
        dve(lambda e: e.tensor_scalar(out=svb[:, 2:3], in0=svb[:, 1:2], scalar1=float(LN_EPS), scalar2=None, op0=ALU.add),
            [("sv", b)], [("sv2", b)])
        act(lambda e: e.activation(out=svb[:, 2:3], in_=svb[:, 2:3], func=AF.Ln), [("sv2", b)], [("sv2", b)])
        act(lambda e: e.activation(out=svb[:, 2:3], in_=svb[:, 2:3], func=AF.Exp, scale=-0.5), [("sv2", b)], [("sv2", b)])
        dve(lambda e: e.tensor_scalar(out=svb[:, 6:7], in0=svb[:, 0:1], scalar1=-1.0, scalar2=None, op0=ALU.mult), [("sv", b)], [("nm", b)])
        act(lambda e: e.activation(out=r[b][:, :], in_=r[b][:, :], func=AF.Identity, bias=svb[:, 6:7]),
            [("r", b, 0), ("r", b, 1), ("nm", b), ("st6", b, 0), ("st6", b, 1)], [("r", b, 0), ("r", b, 1)])
        dve(lambda e: e.scalar_tensor_tensor(out=h1[b][:, :], in0=r[b][:, :], scalar=svb[:, 2:3], in1=lng[:, :],
                                             op0=ALU.mult, op1=ALU.mult), [("r", b, 0), ("r", b, 1), ("sv2", b), "lng"], [("h1", b)])
        pool(lambda e: e.tensor_tensor(out=h1[b][:, :], in0=h1[b][:, :], in1=lnb[:, :], op=ALU.add), [("h1", b), "lnb"], [("h1", b)])
        ph.dma("sp", lambda e: e.dma_start(out=h1d[128 * gt: 128 * gt + 128, :], in_=h1[b][:, :]), reads=[("h1", b)], key=f"h1o{b}")
        for cc in range(8):
            ph.op("pe", (lambda cc: lambda e: e.transpose(out=tp[:, 128 * cc: 128 * cc + 128], in_=h1[b][:, 128 * cc: 128 * cc + 128],
                                                          identity=c.ident_f[:, :]))(cc),
                  reads=[("h1", b)], writes=[("tp", cc // 4)])

    def tileB2(tt):
        gt = 16 * s + tt
        b = tt % 2
        act(lambda e: e.copy(out=h1b[b][:, :], in_=h1[b][:, :]), [("h1", b)], [("h1b", b)])
        act(lambda e: e.copy(out=h1T[b][:, 0:4, :], in_=tp[:, 0:512].rearrange("p (c t) -> p c t", c=4)), [("tp", 0)], [("h1T", b, 0)])
        act(lambda e: e.copy(out=h1T[b][:, 4:8, :], in_=tp[:, 512:1024].rearrange("p (c t) -> p c t", c=4)), [("tp", 1)], [("h1T", b, 1)])
        for cc in range(8):
            mm(lgp[:, :], h1T[b][:, cc, :], wr[:, cc, :], cc == 0, cc == 7, [("h1T", b, cc // 4), "wr"], ["lgp"])
        act(lambda e: e.copy(out=lgraw[b][:, :], in_=lgp[:, :]), ["lgp"], [("lgraw", b)])

    def stageC(tt):
        gt = 16 * s + tt
        b = tt % 2
        svb = sv[b]
        dve(lambda e: e.tensor_tensor(out=lg[b][:, :], in0=lgraw[b][:, :], in1=brt[:, :], op=ALU.add), [("lgraw", b), "brt"], [("lg", b)])
        dve(lambda e: e.max(out=t8[b][:, :], in_=lg[b][:, :]), [("lg", b)], [("t8", b)])
        dve(lambda e: e.tensor_scalar(out=svb[:, 3:4], in0=t8[b][:, 0:1], scalar1=-1.0, scalar2=None, op0=ALU.mult), [("t8", b)], [("nt0", b)])
        act(lambda e: e.activation(out=e4[b][:, :], in_=t8[b][:, 0:4], func=AF.Exp, bias=svb[:, 3:4], accum_out=svb[:, 4:5]),
            [("t8", b), ("nt0", b)], [("e4", b), ("gsum", b)])
        dve(lambda e: e.reciprocal(out=svb[:, 5:6], in_=svb[:, 4:5]), [("gsum", b)], [("rs", b)])
        dve(lambda e: e.tensor_scalar(out=P.gates[:, gt, :], in0=e4[b][:, :], scalar1=svb[:, 5:6], scalar2=None, op0=ALU.mult),
            [("e4", b), ("rs", b)], [("gates", gt)])
        dve(lambda e: e.tensor_scalar(out=Mf[b][:, :], in0=lg[b][:, :], scalar1=t8[b][:, 3:4], scalar2=None, op0=ALU.is_ge),
            [("lg", b), ("t8", b)], [("Mf", b)])
        dve(lambda e: e.tensor_copy(out=Mb[b][:, :], in_=Mf[b][:, :]), [("Mf", b)], [("Mb", b)])
        act(lambda e: e.activation(out=ex[b][:, :], in_=lg[b][:, :], func=AF.Exp, bias=svb[:, 3:4]), [("lg", b), ("nt0", b)], [("ex", b)])
        dve(lambda e: e.scalar_tensor_tensor(out=P.Gd[:, gt, :], in0=ex[b][:, :], scalar=svb[:, 5:6], in1=Mf[b][:, :],
                                             op0=ALU.mult, op1=ALU.mult), [("ex", b), ("rs", b), ("Mf", b)], [("Gd", gt)])
        mm(pfp[:, 0:32], c.tris_bf[:, :], Mb[b][:, :], True, True, [("Mb", b)], ["pfp"])
        mm(pfp[:, 32:64], c.ones_bf[:, :], Mb[b][:, :], True, True, [("Mb", b)], ["pfp"])
        dve(lambda e: e.tensor_tensor(out=rowf[b][:, :], in0=pfp[:, 0:32], in1=P.base[:, :], op=ALU.add), ["pfp", "base"], [("rowf", b)])
        dve(lambda e: e.tensor_tensor(out=P.base[:, :], in0=pfp[:, 32:64], in1=P.base[:, :], op=ALU.add), ["pfp", "base"], ["base"])
        dve(lambda e: e.tensor_scalar(out=rowf[b][:, :], in0=rowf[b][:, :], scalar1=float(CAP - 1), scalar2=None, op0=ALU.min),
            [("rowf", b)], [("rowf", b)])
        dve(lambda e: e.tensor_tensor(out=rowf[b][:, :], in0=rowf[b][:, :], in1=ecap[:, :], op=ALU.add), [("rowf", b), "ecap"], [("rowf", b)])
        for k in range(4):
            (lambda k: dve(lambda e: e.scalar_tensor_tensor(out=j32[b][:, :], in0=lg[b][:, :], scalar=t8[b][:, k: k + 1], in1=rowf[b][:, :],
                                                            op0=ALU.is_equal, op1=ALU.mult, accum_out=rk[b][:, k: k + 1]),
                           [("lg", b), ("t8", b), ("rowf", b)], [("j32", b), ("rk", b, k)]))(k)
        dve(lambda e: e.tensor_copy(out=P.rows[:, gt, :], in_=rk[b][:, :]), [("rk", b, k) for k in range(4)], [("rows", gt)])
        for k in range(4):
            (lambda k: ph.dma("pool", lambda e: e.indirect_dma_start(
                out=Xg, out_offset=bass.IndirectOffsetOnAxis(ap=P.rows[:, gt, k: k + 1], axis=0), in_=h1b[b][:, :], in_offset=None),
                reads=[("rows", gt), ("h1b", b)], key=f"sc{b}_{k}"))(k)

    stage1(0)
    stage1(1)
    tile(0)
    tileB2(0)
    for tt in range(16):
        if tt + 2 < 16:
            stage1(tt + 2)
        if tt + 1 < 16:
            tile(tt + 1)
        stageC(tt)
        if tt + 1 < 16:
            tileB2(tt + 1)
    if dbg is not None:
        ph.dma("sp", lambda e: e.dma_start(out=dbg["rows"], in_=P.rows[:]), reads=[("rows", t) for t in range(32)], key="dbgr")
        ph.dma("sp", lambda e: e.dma_start(out=dbg["gates"], in_=P.gates[:]), reads=[("gates", t) for t in range(32)], key="dbgg")
    ph.close()


NROWS = NEXP * CAP


class Persist:
    pass


def build_program(stop_after=None, debug=False, nseq=NSEQ):
    nc = bass.Bass("TRN2", target_bir_lowering=False)
    dt = lambda name, shape, dtype, kind: nc.dram_tensor(name, list(shape), dtype, kind=kind).ap()
    xT_d = dt("xT", [NSEQ, D, S], F32, "ExternalInput")
    x_d = dt("x", [TOK, D], F32, "ExternalInput")
    w_in = dt("w_in", [D, PW], F32, "ExternalInput")
    wmq = dt("w_mq", [4, 128, 128], F32, "ExternalInput")
    wmk = dt("w_mk", [4, 128, 128], F32, "ExternalInput")
    w_out = dt("w_out", [D, D], F32, "ExternalInput")
    w_router = dt("w_router", [D, NEXP], F32, "ExternalInput")
    hc = host_consts()
    cd = {k[2:]: dt(k, v.shape, F32, "ExternalInput") for k, v in hc.items()}
    sm = {k[2:]: dt(k, shp, F32, "ExternalInput") for k, shp in SMALL_SHAPES.items()}
    wg_d = dt("w_gate", [NEXP, D, D], F32, "ExternalInput")
    wu_d = dt("w_up", [NEXP, D, D], F32, "ExternalInput")
    wd_d = dt("w_down", [NEXP, D, D], F32, "ExternalInput")
    bgT_d = dt("bgT", [128, NEXP * 8], F32, "ExternalInput")
    buT_d = dt("buT", [128, NEXP * 8], F32, "ExternalInput")
    bd_d = dt("b_down", [NEXP, D], F32, "ExternalInput")
    out_d = dt("out", [TOK, D], F32, "ExternalOutput")
    Yg = dt("Yg", [NROWS, D], F32, "Internal")
    scratch_kind = "ExternalOutput" if debug else "Internal"
    h1d = dt("h1d", [TOK, D], F32, scratch_kind)
    Xg = dt("Xg", [NROWS, D], BF16, scratch_kind)
    dbg = None
    if debug:
        dbg = {"rows": dt("dbg_rows", [128, 32, 4], I32, "ExternalOutput"),
               "gates": dt("dbg_gates", [128, 32, 4], F32, "ExternalOutput")}
    top = contextlib.ExitStack()
    c = setup_consts(nc, top, cd)
    P = Persist()
    P.rows = top.enter_context(nc.sbuf_tensor("P_rows", [128, 32, 4], I32))
    P.gates = top.enter_context(nc.sbuf_tensor("P_gates", [128, 32, 4], F32))
    P.Gd = top.enter_context(nc.sbuf_tensor("P_Gd", [128, 32, 32], F32))
    P.base = top.enter_context(nc.sbuf_tensor("P_base", [128, 32], F32))
    for s in range(nseq):
        with contextlib.ExitStack() as sq:
            xT = sq.enter_context(nc.sbuf_tensor(f"xT_sb{s}", [128, 8, S], BF16))
            attnT = sq.enter_context(nc.sbuf_tensor(f"attnT{s}", [64, 8, S], BF16))
            hmT = sq.enter_context(nc.sbuf_tensor(f"hmT{s}", [128, 4, S], BF16))
            attention_phase(nc, c, s, xT_d, w_in, xT, attnT, Xg=(Xg if s == 0 else None))
            mlstm_phase(nc, c, s, w_in, wmq, wmk, sm, xT, hmT)
            outproj_phase(nc, c, s, x_d, w_out, w_router, sm, attnT, hmT, P, h1d, Xg, dbg=dbg)
    if stop_after == "mixer":
        final_cleanup(nc)
        top.close()
        return nc
    experts_phase(nc, c, wg_d, wu_d, wd_d, bgT_d, buT_d, Xg, Yg)
    combine_phase(nc, c, sm, bd_d, P, h1d, Yg, out_d)
    final_cleanup(nc)
    top.close()
    return nc


SMALL_SHAPES = {"s_cwT": (128, 16), "s_cbT": (128, 4), "s_gb": (128, 128), "s_mgT": (128, 4),
                "s_ln1g": (128, D), "s_ln1b": (128, D), "s_ln2g": (128, D), "s_ln2b": (128, D),
                "s_brt": (128, 32), "s_ecap": (128, 32)}


def core_inputs(inputs, core):
    x = inputs["x"][2 * core: 2 * core + 2]
    m = {
        "xT": np.ascontiguousarray(x.transpose(0, 2, 1)),
        "x": np.ascontiguousarray(x.reshape(TOK, D)),
        "w_in": inputs["w_in"][0], "w_mq": inputs["w_mq"][0], "w_mk": inputs["w_mk"][0],
        "w_out": inputs["w_out"][0], "w_router": inputs["w_router"][0],
        "w_gate": inputs["w_gate"][0], "w_up": inputs["w_up"][0], "w_down": inputs["w_down"][0],
        "b_down": inputs["b_down"][0],
    }
    return m


def host_shared(inputs):
    m = dict(host_consts())
    m.update(host_small(inputs))
    tb = lambda b: np.ascontiguousarray(b.reshape(NEXP, 8, 128).transpose(2, 0, 1)).reshape(128, NEXP * 8)
    m["bgT"] = tb(inputs["b_gate"][0])
    m["buT"] = tb(inputs["b_up"][0])
    return m


_NC_CACHE = {}


def kernel(**inputs):
    inputs = {k: np.asarray(v) for k, v in inputs.items()}
    if "nc" not in _NC_CACHE:
        _NC_CACHE["nc"] = build_program()
    nc = _NC_CACHE["nc"]
    shared = host_shared(inputs)
    in_maps = []
    for core in range(8):
        m = core_inputs(inputs, core)
        m.update(shared)
        in_maps.append(m)
    res = run_bass_kernel_spmd(nc, in_maps, core_ids=list(range(8)))
    out = np.concatenate([r["out"].reshape(NSEQ, S, D) for r in res.results], axis=0)
    return out.astype(np.float32)


def experts_phase(nc, c, wg_d, wu_d, wd_d, bgT_d, buT_d, Xg, Yg, nexp=NEXP):
    ph = Phase(nc, "ex")
    W = [[ph.sb(f"W{i}_{m}", [128, 8, D], BF16) for m in range(3)] for i in range(2)]
    wsrc = [wg_d, wu_d, wd_d]
    bg = ph.sb("bg", [128, NEXP * 8], F32)
    bu1 = ph.sb("bu1", [128, NEXP * 8], F32)
    ph.dma("sp", lambda e: e.dma_start(out=bg[:], in_=bgT_d), writes=["bg"], key="bg")
    ph.dma("sp", lambda e: e.dma_start(out=bu1[:], in_=buT_d), writes=["bu1"], key="bu1")
    ph.op("dve", lambda e: e.tensor_scalar(out=bu1[:], in0=bu1[:], scalar1=1.0, scalar2=None, op0=ALU.add), reads=["bu1"], writes=["bu1"])
    xg = [ph.sb(f"xg{i}", [128, 5, D], BF16) for i in range(2)]
    XgT = [ph.sb(f"XgT{i}", [128, 8, CAP], BF16) for i in range(2)]
    HT = [ph.sb(f"HT{i}", [128, 8, CAP], BF16) for i in range(2)]
    g1 = [ph.sb(f"g1{i}", [128, 384], F32) for i in range(2)]
    sg = [ph.sb(f"sg{i}", [128, 384], F32) for i in range(2)]
    tu = [ph.sb(f"tu{i}", [128, 384], F32) for i in range(2)]
    ysb = [ph.sb(f"ysb{i}", [128, 512], F32) for i in range(4)]
    tpp = [ph.ps(f"tpp{i}", [128, 1024], BF16) for i in range(2)]
    gp = [ph.ps(f"gp{i}", [128, 384], F32) for i in range(2)]
    up = [ph.ps(f"up{i}", [128, 384], F32) for i in range(2)]
    yp = [ph.ps(f"yp{i}", [128, 512], F32) for i in range(2)]
    cnt = {"tp": 0, "gu": 0, "y": 0, "ys": 0, "ev": 0}

    def mm(out, lhsT, rhs, start, stop, reads, writes):
        ph.op("pe", lambda e: e.matmul(out, lhsT=lhsT, rhs=rhs, start=start, stop=stop), reads=reads, writes=writes)

    def dve(fn, reads, writes):
        ph.op("dve", fn, reads=reads, writes=writes)

    def act(fn, reads, writes):
        ph.op("act", fn, reads=reads, writes=writes)

    def load_w(ex):
        wi = ex % 2
        for m in range(3):
            src = wsrc[m][ex].rearrange("(c p) n -> p c n", p=128)
            for cc in range(8):
                (lambda m, cc, src: ph.dma("pool", lambda e: e.dma_start(out=W[wi][m][:, cc, :], in_=src[:, cc, :]),
                                           writes=[("W", wi, m, cc)], key=f"W{wi}_{m}_{cc}"))(m, cc, src)

    def load_x(ex):
        xi = ex % 2
        ph.dma("sp", lambda e: e.dma_start(out=xg[xi][:], in_=Xg[ex * CAP:(ex + 1) * CAP, :].rearrange("(j p) d -> p j d", p=128)),
               writes=[("xg", xi)], key=f"xg{xi}")

    def transposes(ex):
        xi = ex % 2
        for fc in range(8):
            ti = cnt["tp"] % 2
            cnt["tp"] += 1
            for j in range(5):
                (lambda fc, j, ti: ph.op("pe", lambda e: e.transpose(out=tpp[ti][:, 128 * j: 128 * j + 128],
                                                                     in_=xg[xi][:, j, 128 * fc: 128 * fc + 128],
                                                                     identity=c.ident_bf[:, :]),
                                         reads=[("xg", xi)], writes=[("tpp", ti)]))(fc, j, ti)
            cnt["ev"] += 1
            if cnt["ev"] % 2 == 0:
                (lambda fc, ti: act(lambda e: e.copy(out=XgT[xi][:, fc, :], in_=tpp[ti][:, 0:CAP]), [("tpp", ti)], [("XgT", xi, fc)]))(fc, ti)
            else:
                (lambda fc, ti: dve(lambda e: e.tensor_copy(out=XgT[xi][:, fc, :], in_=tpp[ti][:, 0:CAP]), [("tpp", ti)], [("XgT", xi, fc)]))(fc, ti)

    def mm1(ex):
        wi = ex % 2
        xi = ex % 2
        for (p0, n) in ((0, 384), (384, 256)):
            psl = slice(p0, p0 + n)
            for fc in range(8):
                gi = cnt["gu"] % 2
                cnt["gu"] += 1
                fsl = slice(128 * fc, 128 * fc + 128)
                for kc in range(8):
                    mm(gp[gi][:, 0:n], W[wi][0][:, kc, fsl], XgT[xi][:, kc, psl], kc == 0, kc == 7,
                       [("W", wi, 0, kc), ("XgT", xi, kc)], [("gp", gi)])
                for kc in range(8):
                    mm(up[gi][:, 0:n], W[wi][1][:, kc, fsl], XgT[xi][:, kc, psl], kc == 0, kc == 7,
                       [("W", wi, 1, kc), ("XgT", xi, kc)], [("up", gi)])

                def epi(gi, fc, n, psl):
                    bcol = slice(ex * 8 + fc, ex * 8 + fc + 1)
                    dve(lambda e: e.tensor_scalar(out=g1[gi][:, 0:n], in0=gp[gi][:, 0:n], scalar1=bg[:, bcol], scalar2=7.0,
                                                  op0=ALU.add, op1=ALU.min), [("gp", gi), "bg"], [("g1", gi)])
                    act(lambda e: e.activation(out=sg[gi][:, 0:n], in_=g1[gi][:, 0:n], func=AF.Sigmoid, scale=1.702),
                        [("g1", gi)], [("sg", gi)])
                    dve(lambda e: e.tensor_scalar(out=tu[gi][:, 0:n], in0=up[gi][:, 0:n], scalar1=bu1[:, bcol], scalar2=-6.0,
                                                  op0=ALU.add, op1=ALU.max), [("up", gi), "bu1"], [("tu", gi)])
                    dve(lambda e: e.tensor_tensor(out=g1[gi][:, 0:n], in0=g1[gi][:, 0:n], in1=sg[gi][:, 0:n], op=ALU.mult),
                        [("g1", gi), ("sg", gi)], [("g1", gi)])
                    dve(lambda e: e.scalar_tensor_tensor(out=HT[xi][:, fc, psl], in0=tu[gi][:, 0:n], scalar=8.0, in1=g1[gi][:, 0:n],
                                                         op0=ALU.min, op1=ALU.mult), [("tu", gi), ("g1", gi)], [("HT", xi, fc)])
                epi(gi, fc, n, psl)

    def mm2(ex):
        wi = ex % 2
        xi = ex % 2
        for j in range(5):
            for nh in range(2):
                yi = cnt["y"] % 2
                cnt["y"] += 1
                for fc in range(8):
                    mm(yp[yi][:, :], HT[xi][:, fc, 128 * j: 128 * j + 128], W[wi][2][:, fc, 512 * nh: 512 * nh + 512],
                       fc == 0, fc == 7, [("HT", xi, fc), ("W", wi, 2, fc)], [("yp", yi)])
                si = cnt["ys"] % 4
                cnt["ys"] += 1
                cnt["ev"] += 1
                if cnt["ev"] % 2 == 0:
                    (lambda yi, si: act(lambda e: e.copy(out=ysb[si][:, :], in_=yp[yi][:, :]), [("yp", yi)], [("ysb", si)]))(yi, si)
                else:
                    (lambda yi, si: dve(lambda e: e.tensor_copy(out=ysb[si][:, :], in_=yp[yi][:, :]), [("yp", yi)], [("ysb", si)]))(yi, si)
                r0 = ex * CAP + 128 * j
                (lambda si, r0, nh: ph.dma("sp", lambda e: e.dma_start(out=Yg[r0: r0 + 128, 512 * nh: 512 * nh + 512], in_=ysb[si][:, :]),
                                           reads=[("ysb", si)], key=f"ys{si}"))(si, r0, nh)

    load_w(0)
    load_x(0)
    transposes(0)
    for ex in range(nexp):
        if ex + 1 < nexp:
            load_w(ex + 1)
            load_x(ex + 1)
        mm1(ex)
        if ex + 1 < nexp:
            transposes(ex + 1)
        mm2(ex)
    ph.close()


def combine_phase(nc, c, sm, bd_d, P, h1d, Yg, out_d, ntiles=32):
    ph = Phase(nc, "cb")
    lng = ph.sb("lng", [128, D], F32)
    lnb = ph.sb("lnb", [128, D], F32)
    bd = ph.sb("bd", [32, D], F32)
    ph.dma("sp", lambda e: e.dma_start(out=lng[:], in_=sm["ln2g"]), writes=["lng"], key="lng")
    ph.dma("sp", lambda e: e.dma_start(out=lnb[:], in_=sm["ln2b"]), writes=["lnb"], key="lnb")
    ph.dma("sp", lambda e: e.dma_start(out=bd[:], in_=bd_d), writes=["bd"], key="bd")
    yk = [[ph.sb(f"yk{i}_{k}", [128, D], F32) for k in range(4)] for i in range(3)]
    h1 = [ph.sb(f"h1{i}", [128, D], F32) for i in range(3)]
    acc = [ph.sb(f"acc{i}", [128, D], F32) for i in range(2)]
    tmp = [ph.sb(f"tmp{i}", [128, D], F32) for i in range(2)]
    gdT = [ph.sb(f"gdT{i}", [32, 128], F32) for i in range(2)]
    st6 = [ph.sb(f"st6{i}", [128, 2, 6], F32) for i in range(2)]
    sv = [ph.sb(f"sv{i}", [128, 4], F32) for i in range(2)]
    gtp = ph.ps("gtp", [32, 128], F32)
    bp = [ph.ps(f"bp{i}", [128, D], F32) for i in range(2)]

    def mm(out, lhsT, rhs, start, stop, reads, writes):
        ph.op("pe", lambda e: e.matmul(out, lhsT=lhsT, rhs=rhs, start=start, stop=stop), reads=reads, writes=writes)

    def dve(fn, reads, writes):
        ph.op("dve", fn, reads=reads, writes=writes)

    def act(fn, reads, writes):
        ph.op("act", fn, reads=reads, writes=writes)

    def pool(fn, reads, writes):
        ph.op("pool", fn, reads=reads, writes=writes)

    dg = [[ph.sb(f"dg{i}_{k}", [128, 128], F32) for k in range(4)] for i in range(2)]

    def load(gt):
        b = gt % 3
        for k in range(4):
            (lambda k: ph.dma("pool", lambda e: e.indirect_dma_start(
                out=yk[b][k][:, :], out_offset=None, in_=Yg,
                in_offset=bass.IndirectOffsetOnAxis(ap=P.rows[:, gt, k: k + 1], axis=0)),
                writes=[("yk", b, k)], key=f"yk{b}_{k}"))(k)
        ph.dma("sp", lambda e: e.dma_start(out=h1[b][:, :], in_=h1d[128 * gt: 128 * gt + 128, :]), writes=[("h1", b)], key=f"h1{b}")

    def front(gt):
        b = gt % 2
        b3 = gt % 3
        for k in range(3):
            (lambda k: dve(lambda e: e.tensor_scalar(out=dg[b][k][:, :], in0=c.ident_f[:, :], scalar1=P.gates[:, gt, k: k + 1], scalar2=None,
                                                     op0=ALU.mult), [], [("dg", b, k)]))(k)
        act(lambda e: e.activation(out=yk[b3][3][:, :], in_=yk[b3][3][:, :], func=AF.Copy, scale=P.gates[:, gt, 3:4]), [("yk", b3, 3)], [("yk", b3, 3)])
        dve(lambda e: e.scalar_tensor_tensor(out=h1[b3][:, :], in0=h1[b3][:, :], scalar=float(ALPHA), in1=yk[b3][3][:, :],
                                             op0=ALU.mult, op1=ALU.add), [("h1", b3), ("yk", b3, 3)], [("h1", b3)])
        ph.op("pe", lambda e: e.transpose(out=gtp[:, :], in_=P.Gd[:, gt, :], identity=c.ident_f[:, :]), reads=[], writes=["gtp"])
        act(lambda e: e.copy(out=gdT[b][:, :], in_=gtp[:, :]), ["gtp"], [("gdT", b)])
        for nh in range(2):
            nsl = slice(512 * nh, 512 * nh + 512)
            mm(bp[b][:, nsl], gdT[b][:, :], bd[:, nsl], True, False, [("gdT", b), "bd"], [("bp", b, nh)])
            for k in range(3):
                mm(bp[b][:, nsl], dg[b][k][:, :], yk[b3][k][:, nsl], False, k == 2, [("dg", b, k), ("yk", b3, k)], [("bp", b, nh)])

    def back(gt):
        b = gt % 2
        b3 = gt % 3
        for nh in range(2):
            nsl = slice(512 * nh, 512 * nh + 512)
            (lambda nsl, nh: dve(lambda e: e.tensor_tensor(out=acc[b][:, nsl], in0=h1[b3][:, nsl], in1=bp[b][:, nsl], op=ALU.add),
                                 [("h1", b3), ("bp", b, nh)], [("acc", b, nh)]))(nsl, nh)
            (lambda nh: dve(lambda e: e.bn_stats(out=st6[b][:, nh, :], in_=acc[b][:, 512 * nh: 512 * nh + 512]), [("acc", b, nh)], [("st6", b, nh)]))(nh)
        accr = [("acc", b, 0), ("acc", b, 1)]
        svb = sv[b]
        dve(lambda e: e.bn_aggr(out=svb[:, 0:2], in_=st6[b][:, :, :].rearrange("p a b -> p (a b)")), [("st6", b, 0), ("st6", b, 1)], [("sv", b)])
        dve(lambda e: e.tensor_scalar(out=svb[:, 2:3], in0=svb[:, 1:2], scalar1=float(LN_EPS), scalar2=None, op0=ALU.add), [("sv", b)], [("sv2", b)])
        act(lambda e: e.activation(out=svb[:, 2:3], in_=svb[:, 2:3], func=AF.Ln), [("sv2", b)], [("sv2", b)])
        act(lambda e: e.activation(out=svb[:, 2:3], in_=svb[:, 2:3], func=AF.Exp, scale=-0.5), [("sv2", b)], [("sv2", b)])
        dve(lambda e: e.tensor_scalar(out=svb[:, 3:4], in0=svb[:, 0:1], scalar1=-1.0, scalar2=None, op0=ALU.mult), [("sv", b)], [("nm", b)])
        act(lambda e: e.activation(out=acc[b][:, :], in_=acc[b][:, :], func=AF.Identity, bias=svb[:, 3:4]),
            accr + [("nm", b), ("st6", b, 0), ("st6", b, 1)], accr)
        dve(lambda e: e.scalar_tensor_tensor(out=tmp[b][:, :], in0=acc[b][:, :], scalar=svb[:, 2:3], in1=lng[:, :],
                                             op0=ALU.mult, op1=ALU.mult), accr + [("sv2", b), "lng"], [("tmp", b)])
        pool(lambda e: e.tensor_tensor(out=tmp[b][:, :], in0=tmp[b][:, :], in1=lnb[:, :], op=ALU.add), [("tmp", b), "lnb"], [("tmp", b)])
        ph.dma("sp", lambda e: e.dma_start(out=out_d[128 * gt: 128 * gt + 128, :], in_=tmp[b][:, :]), reads=[("tmp", b)], key=f"out{b}")

    load(0)
    if ntiles > 1:
        load(1)
    front(0)
    for gt in range(ntiles):
        if gt + 2 < ntiles:
            load(gt + 2)
        if gt + 1 < ntiles:
            front(gt + 1)
        back(gt)
    ph.close()
```

```python
import contextlib
import numpy as np
import concourse.bass as bass
import concourse.mybir as mybir
from concourse.bass_utils import run_bass_kernel_spmd

F32 = mybir.dt.float32
BF16 = mybir.dt.bfloat16
I32 = mybir.dt.int32
U32 = mybir.dt.uint32
AF = mybir.ActivationFunctionType
ALU = mybir.AluOpType
AX = mybir.AxisListType

ENGS = ("pe", "act", "dve", "pool", "sp")


class _Op:
    __slots__ = ("eng", "fn", "reads", "writes", "dma", "key", "idx", "deps",
                 "need_inc", "cnt", "sem")

    def __init__(self, eng, fn, reads, writes, dma, key):
        self.eng, self.fn, self.reads, self.writes = eng, fn, reads, writes
        self.dma, self.key = dma, key
        self.deps = []
        self.need_inc = False
        self.cnt = 0
        self.sem = None


_BAR = {}


def init_barrier(nc, top):
    _BAR[id(nc)] = {"done": top.enter_context(nc.semaphore("bar_done")),
                    "clr": top.enter_context(nc.semaphore("bar_clr")), "n": 0}


def final_cleanup(nc):
    bar = _BAR[id(nc)]
    n = bar["n"]
    with nc.semaphore("bar_fin") as fin:
        with nc.Block() as block:
            def body(eng, is_sp):
                eng.sem_inc(fin, 1)
                if is_sp:
                    eng.wait_ge(fin, 5)
                    eng.sem_clear(bar["done"])
                    eng.sem_clear(bar["clr"])
                    eng.sem_clear(fin)

            @block.tensor
            def _(e):
                body(e, False)

            @block.scalar
            def _(e):
                body(e, False)

            @block.vector
            def _(e):
                body(e, False)

            @block.gpsimd
            def _(e):
                body(e, False)

            @block.sync
            def _(e):
                body(e, True)


class Phase:
    def __init__(self, nc, name="ph", same_engine_sync=True):
        self.nc = nc
        self.name = name
        self.ops = []
        self.stack = contextlib.ExitStack()
        self.same_engine_sync = same_engine_sync

    def sb(self, name, shape, dt):
        return self.stack.enter_context(self.nc.sbuf_tensor(f"{self.name}_{name}", list(shape), dt))

    def ps(self, name, shape, dt):
        return self.stack.enter_context(self.nc.psum_tensor(f"{self.name}_{name}", list(shape), dt))

    def op(self, eng, fn, reads=(), writes=(), dma=False, key=None):
        o = _Op(eng, fn, tuple(reads), tuple(writes), dma, key)
        o.idx = len(self.ops)
        self.ops.append(o)
        return o

    def dma(self, eng, fn, reads=(), writes=(), key=None):
        assert key is not None
        return self.op(eng, fn, reads, writes, dma=True, key=key)

    def close(self):
        nc = self.nc
        ops = self.ops
        last_w = {}
        readers = {}
        for o in ops:
            deps = set()
            for t in o.reads:
                w = last_w.get(t)
                if w is not None:
                    deps.add(w)
            for t in o.writes:
                w = last_w.get(t)
                if w is not None:
                    deps.add(w)
                for r in readers.get(t, ()):
                    deps.add(r)
            for t in o.reads:
                readers.setdefault(t, []).append(o)
            for t in o.writes:
                last_w[t] = o
                readers[t] = []
            deps.discard(o)
            dl = []
            for d in deps:
                if (not d.dma) and d.eng == o.eng and (o.eng == "pe" or not self.same_engine_sync):
                    continue
                dl.append(d)
                d.need_inc = True
            o.deps = dl
        sems = {}

        def getsem(k):
            if k not in sems:
                sems[k] = self.stack.enter_context(nc.semaphore(f"{self.name}_{k}"))
            return sems[k]

        cnts = {}
        for o in ops:
            if o.dma:
                k = ("d", o.key)
                o.sem = k
                cnts[k] = cnts.get(k, 0) + 16
                o.cnt = cnts[k]
            elif o.need_inc:
                k = ("e", o.eng)
                o.sem = k
                cnts[k] = cnts.get(k, 0) + 1
                o.cnt = cnts[k]
        for k in cnts:
            getsem(str(k[0]) + "_" + str(k[1]))
        final = dict(cnts)
        swd = set()
        for o in ops:
            if o.dma and o.eng == "pool":
                swd.add(str(o.sem[0]) + "_" + str(o.sem[1]))
            if o.dma:
                assert (o.eng == "pool") == ((str(o.sem[0]) + "_" + str(o.sem[1])) in swd), ("mixed SW/HW DGE on one key", o.key)
        per_eng = {e: [o for o in ops if o.eng == e] for e in ENGS}

        def emit(engname, eng):
            waited = {}
            for o in per_eng[engname]:
                need = {}
                for d in o.deps:
                    if need.get(d.sem, 0) < d.cnt:
                        need[d.sem] = d.cnt
                for k, v in need.items():
                    if waited.get(k, 0) >= v:
                        continue
                    eng.wait_ge(getsem(str(k[0]) + "_" + str(k[1])), v)
                    waited[k] = v
                ins = o.fn(eng)
                if o.dma:
                    ins.then_inc(getsem(str(o.sem[0]) + "_" + str(o.sem[1])), 16)
                elif o.need_inc:
                    ins.then_inc(getsem(str(o.sem[0]) + "_" + str(o.sem[1])), 1)
            for k, v in final.items():
                if waited.get(k, 0) >= v:
                    continue
                eng.wait_ge(getsem(str(k[0]) + "_" + str(k[1])), v)
            eng.sem_inc(bar["done"], 1)
            if engname == "pool":
                eng.wait_ge(bar["done"], 5 * (kph + 1))
                nums = sorted(sems[sn].num for sn in swd)
                i = 0
                while i < len(nums):
                    j = i
                    while j + 1 < len(nums) and nums[j + 1] == nums[j] + 1:
                        j += 1
                    eng.dma_reset(range(nums[i], nums[j] + 1))
                    i = j + 1
                for sname in sorted(sems):
                    eng.sem_clear(sems[sname])
                eng.sem_inc(bar["clr"], 1)
            eng.wait_ge(bar["clr"], kph + 1)

        bar = _BAR[id(nc)]
        kph = bar["n"]
        bar["n"] += 1
        with nc.Block() as block:
            @block.tensor
            def _(e):
                emit("pe", e)

            @block.scalar
            def _(e):
                emit("act", e)

            @block.vector
            def _(e):
                emit("dve", e)

            @block.gpsimd
            def _(e):
                emit("pool", e)

            @block.sync
            def _(e):
                emit("sp", e)
        self.stack.close()
        self.ops = []


D = 1024
S = 2048
NSEQ = 2
TOK = NSEQ * S
PW = 3080
NEXP = 32
CAP = 640
NEG = -30000.0
ALPHA = 2.0 ** 0.25
LN_EPS = 1e-5
RMS_EPS = 1e-6


class Consts:
    pass


def setup_consts(nc, top, cdram):
    init_barrier(nc, top)
    c = Consts()
    c.ident_bf = top.enter_context(nc.sbuf_tensor("ident_bf", [128, 128], BF16))
    c.ident_f = top.enter_context(nc.sbuf_tensor("ident_f", [128, 128], F32))
    c.maskb = top.enter_context(nc.sbuf_tensor("maskb", [128, 256], BF16))
    c.maskA = top.enter_context(nc.sbuf_tensor("maskA", [128, 512], BF16))
    c.maskB = top.enter_context(nc.sbuf_tensor("maskB", [128, 512], BF16))
    c.ones_f = top.enter_context(nc.sbuf_tensor("ones_f", [128, 128], F32))
    c.ones_bf = top.enter_context(nc.sbuf_tensor("ones_bf", [128, 128], BF16))
    c.tri_bf = top.enter_context(nc.sbuf_tensor("tri_bf", [128, 128], BF16))
    c.tris_bf = top.enter_context(nc.sbuf_tensor("tris_bf", [128, 128], BF16))
    c.tri_f = top.enter_context(nc.sbuf_tensor("tri_f", [128, 128], F32))
    c.tri4_bf = top.enter_context(nc.sbuf_tensor("tri4_bf", [128, 512], BF16))
    ph = Phase(nc, "cst")
    ph.dma("sp", lambda e: e.dma_start(out=c.ident_f[:], in_=cdram["ident"]), writes=["a"], key="a")
    ph.dma("sp", lambda e: e.dma_start(out=c.ones_f[:], in_=cdram["ones"]), writes=["b"], key="b")
    ph.dma("sp", lambda e: e.dma_start(out=c.tri_f[:], in_=cdram["tri"]), writes=["c"], key="c")
    ph.dma("pool", lambda e: e.dma_start(out=c.ident_bf[:], in_=cdram["ident"]), writes=["d"], key="d")
    ph.dma("pool", lambda e: e.dma_start(out=c.maskb[:], in_=cdram["maskb"]), writes=["e"], key="e")
    ph.dma("pool", lambda e: e.dma_start(out=c.maskA[:], in_=cdram["maskA"]), writes=["e01"], key="e01")
    ph.dma("pool", lambda e: e.dma_start(out=c.maskB[:], in_=cdram["maskB"]), writes=["e02"], key="e02")
    ph.dma("pool", lambda e: e.dma_start(out=c.ones_bf[:], in_=cdram["ones"]), writes=["f"], key="f")
    ph.dma("pool", lambda e: e.dma_start(out=c.tri_bf[:], in_=cdram["tri"]), writes=["g"], key="g")
    ph.dma("pool", lambda e: e.dma_start(out=c.tris_bf[:], in_=cdram["tris"]), writes=["h"], key="h")
    ph.dma("pool", lambda e: e.dma_start(out=c.tri4_bf[:], in_=cdram["tri4"]), writes=["i4"], key="i4")
    ph.close()
    return c


def host_consts():
    k = np.arange(128)[:, None]
    q = np.arange(128)[None, :]
    maskb = np.zeros((128, 256), np.float32)
    maskb[:, 0:128] = np.where(k <= q, 0.0, NEG)
    maskb[:, 128:256] = np.where(k >= q, 0.0, NEG)
    return {
        "c_ident": np.eye(128, dtype=np.float32),
        "c_ones": np.ones((128, 128), np.float32),
        "c_tri": (k <= q).astype(np.float32),
        "c_tris": (k < q).astype(np.float32),
        "c_tri4": np.tile((k <= q).astype(np.float32), (1, 4)),
        "c_maskb": maskb,
        "c_maskA": np.tile((maskb == 0.0).astype(np.float32), (1, 2)),
        "c_maskB": np.tile((maskb[:, 0:128] == 0.0).astype(np.float32), (1, 4)),
    }


def attention_phase(nc, c, s, xT_dram, w_in, xT, attnT, dbg=None, Xg=None):
    ph = Phase(nc, f"at{s}")
    wqkv = ph.sb("wqkv", [128, 8, 1536], BF16)
    w_in_v = w_in.rearrange("(c p) n -> p c n", p=128)
    xT_v = xT_dram[s].rearrange("(c p) t -> p c t", p=128)

    def ld(cc):
        ph.dma("pool", lambda e: e.dma_start(out=xT[:, cc, :], in_=xT_v[:, cc, :]),
               writes=[("xT", cc)], key=f"xT{cc}")
        ph.dma("pool", lambda e: e.dma_start(out=wqkv[:, cc, :], in_=w_in_v[:, cc, 0:1536]),
               writes=[("wqkv", cc)], key=f"wq{cc}")
    for cc in range(8):
        ld(cc)
    if Xg is not None:
        zt = ph.sb("zt", [128, 8192], BF16)
        ph.op("dve", lambda e: e.memset(zt[:], 0.0), writes=["zt"])
        Xg_v = Xg.rearrange("(p r) d -> p (r d)", p=128)
        for i in range(NROWS // 128 * D // 8192):
            (lambda i: ph.dma("sp", lambda e: e.dma_start(out=Xg_v[:, 8192 * i: 8192 * (i + 1)], in_=zt[:]), reads=["zt"], key="zx"))(i)
    QT = [ph.sb(f"QT{i}", [128, S], BF16) for i in range(2)]
    KT = [ph.sb(f"KT{i}", [128, S], BF16) for i in range(2)]
    V3 = [[ph.sb(f"V{i}_{l}", [128, 16, 2, 65], BF16) for l in range(3)] for i in range(2)]

    def ms(t, tok):
        ph.op("pool", lambda e: e.memset(t[:], 1.0), writes=[tok])
    for i in range(2):
        for l in range(3):
            ms(V3[i][l], ("V", i, l))
    pj = [ph.ps(f"pj{i}", [128, 512], F32) for i in range(2)]
    st = [ph.ps(f"st{i}", [128, 512], F32) for i in range(2)]
    acc = ph.ps("acc", [128, S], F32)
    pt = [ph.sb(f"pt{i}", [128, 512], BF16) for i in range(4)]
    pe = [ph.sb(f"pe{i}", [128, 512], BF16) for i in range(4)]
    rc = ph.sb("rc", [128, S], F32)
    bcs = [ph.sb(f"bcs{i}", [64, 512], F32) for i in range(2)]
    cnt = {"pj": 0, "st": 0, "pt": 0, "ev": 0}

    def evac(out, in_, reads, writes):
        cnt["ev"] += 1
        if cnt["ev"] % 3 == 0:
            ph.op("act", lambda e: e.copy(out=out, in_=in_), reads=reads, writes=writes)
        else:
            ph.op("dve", lambda e: e.tensor_copy(out=out, in_=in_), reads=reads, writes=writes)

    def mm(out, lhsT, rhs, start, stop, reads, writes, skip=False):
        ph.op("pe", lambda e: e.matmul(out, lhsT=lhsT, rhs=rhs, start=start, stop=stop, skip_group_check=skip),
              reads=reads, writes=writes)

    def nextpj():
        i = cnt["pj"] % 2
        cnt["pj"] += 1
        return pj[i], ("pj", i)

    sbufs = [(st[0], ("st", 0)), (st[1], ("st", 1)), (pj[0], ("pj", 0)), (pj[1], ("pj", 1))]

    def score_group(bi, pr, blocks, mask):
        sb_, stok = sbufs[cnt["st"] % 4]
        cnt["st"] += 1
        qk_reads = [("QT", bi, n) for n in range(4)] + [("KT", bi, n) for n in range(4)]
        off = 0
        offs = []
        for (ksl, qsl, nq, pvs) in blocks:
            mm(sb_[:, off:off + nq], KT[bi][pr, ksl], QT[bi][pr, qsl], True, True, qk_reads, [stok])
            offs.append(off)
            off += nq
        tot = off
        pi = cnt["pt"] % 4
        cnt["pt"] += 1
        ph.op("act", lambda e: e.activation(out=pe[pi][:, 0:tot], in_=sb_[:, 0:tot], func=AF.Exp, scale=0.125),
              reads=[stok], writes=[("pe", pi)])
        ph.op("dve", lambda e: e.tensor_tensor(out=pt[pi][:, 0:tot], in0=pe[pi][:, 0:tot], in1=mask[:, 0:tot], op=ALU.mult),
              reads=[("pe", pi)], writes=[("pt", pi)])
        return pi, offs

    def norm(h):
        for b in range(4):
            (lambda bsl: ph.op("act", lambda e: e.activation(out=rc[64:65, bsl], in_=acc[64:65, bsl], func=AF.Ln),
                               reads=[("acc", b)], writes=[("rc", b)]))(slice(512 * b, 512 * b + 512))
            (lambda bsl: ph.op("act", lambda e: e.activation(out=rc[64:65, bsl], in_=rc[64:65, bsl], func=AF.Exp, scale=-1.0),
                               reads=[("rc", b)], writes=[("rc", b)]))(slice(512 * b, 512 * b + 512))
        for b in range(4):
            bsl = slice(512 * b, 512 * b + 512)
            p, ptok = nextpj()
            bi2 = cnt["ev"] % 2
            cnt["ev"] += 1
            mm(p[0:64, :], c.ones_f[64:65, 0:64], rc[64:65, bsl], True, True, [("rc", b)], [ptok])
            (lambda p, bi2: ph.op("act", lambda e: e.copy(out=bcs[bi2][:, :], in_=p[0:64, :]), reads=[ptok], writes=[("bcs", bi2)]))(p, bi2)
            (lambda bsl, bi2: ph.op("dve", lambda e: e.tensor_tensor(out=attnT[:, h, bsl], in0=acc[0:64, bsl], in1=bcs[bi2][:, :], op=ALU.mult),
                                    reads=[("bcs", bi2), ("acc", b)], writes=[("attnT", h, b), ("acc", b)]))(bsl, bi2)

    for a in range(4):
        bi = a % 2
        for (dst, col0, nm) in ((QT[bi], 0, "QT"), (KT[bi], 512, "KT")):
            for n in range(4):
                p, ptok = nextpj()
                for cc in range(8):
                    mm(p[:, :], wqkv[:, cc, col0 + 128 * a: col0 + 128 * a + 128], xT[:, cc, 512 * n: 512 * n + 512],
                       cc == 0, cc == 7, [("xT", cc), ("wqkv", cc)], [ptok])
                evac(dst[:, 512 * n: 512 * n + 512], p[:, :], [ptok], [(nm, bi, n)])
        for l, dil in enumerate((1, 4, 16)):
            for g0 in range(0, 16, 4):
                p, ptok = nextpj()
                for gi in range(4):
                    g = g0 + gi
                    if dil == 1:
                        tsl = slice(128 * g, 128 * g + 128)
                    elif dil == 4:
                        r, n = g // 4, g % 4
                        tsl = slice(512 * n + r, 512 * n + r + 509, 4)
                    else:
                        tsl = slice(g, g + 2033, 16)
                    for cc in range(8):
                        mm(p[:, 128 * gi: 128 * gi + 128], xT[:, cc, tsl],
                           wqkv[:, cc, 1024 + 128 * a: 1024 + 128 * a + 128], cc == 0, cc == 7,
                           [("xT", cc), ("wqkv", cc)], [ptok])
                evac(V3[bi][l][:, g0:g0 + 4, :, 0:64],
                     p[:, :].rearrange("p (g j e) -> p g j e", g=4, j=2), [ptok], [("V", bi, l)])
        tasks = []
        for j in range(2):
            h = 2 * a + j
            pr = slice(64 * j, 64 * j + 64)
            b1 = []
            for n in range(16):
                nq = 256 if n < 15 else 128
                pvs = [(slice(128 * (qb - n), 128 * (qb - n) + 128), V3[bi][0][:, n, j, :], slice(128 * qb, 128 * qb + 128), qb // 4)
                       for qb in range(n, min(n + 2, 16))]
                b1.append((slice(128 * n, 128 * n + 128), slice(128 * n, 128 * n + nq), nq, pvs))
            for g in range(0, 16, 2):
                tasks.append(("grp", h, pr, b1[g:g + 2], c.maskA))
            for r in range(4):
                b2 = []
                for n in range(4):
                    nq = 256 if n < 3 else 128
                    b0 = 512 * n + r
                    pvs = [(slice(128 * (qb - n), 128 * (qb - n) + 128), V3[bi][1][:, 4 * r + n, j, :],
                            slice(512 * qb + r, 512 * qb + r + 509, 4), qb) for qb in range(n, min(n + 2, 4))]
                    b2.append((slice(b0, b0 + 509, 4), slice(b0, b0 + 4 * nq - 3, 4), nq, pvs))
                tasks.append(("grp", h, pr, b2[0:2], c.maskA))
                tasks.append(("grp", h, pr, b2[2:4], c.maskA))
            b3 = []
            for r in range(16):
                pvs = [(slice(32 * b, 32 * b + 32), V3[bi][2][:, r, j, :], slice(512 * b + r, 512 * b + r + 497, 16), b) for b in range(4)]
                b3.append((slice(r, r + 2033, 16), slice(r, r + 2033, 16), 128, pvs))
            for g in range(0, 16, 4):
                tasks.append(("grp", h, pr, b3[g:g + 4], c.maskB))
            tasks.append(("norm", h))
        started = {}
        vreads = [("V", bi, 0), ("V", bi, 1), ("V", bi, 2)]

        def emit_pv(t, pi, offs):
            h = t[1]
            for (blk, off) in zip(t[3], offs):
                for (psl, vap, osl, bank) in blk[3]:
                    first = not started.get((h, bank), False)
                    started[(h, bank)] = True
                    mm(acc[0:65, osl], vap, pt[pi][:, off + psl.start: off + psl.stop], first, True, [("pt", pi)] + vreads,
                       [("acc", bank)], skip=True)

        pending = []
        for t in tasks:
            if t[0] == "grp":
                pi, offs = score_group(bi, t[2], t[3], t[4])
                pending.append((t, pi, offs))
                if len(pending) > 2:
                    emit_pv(*pending.pop(0))
            else:
                while pending:
                    emit_pv(*pending.pop(0))
                norm(t[1])
    if dbg is not None:
        ph.dma("sp", lambda e: e.dma_start(out=dbg, in_=attnT[:]),
               reads=[("attnT", h, b) for h in range(8) for b in range(4)], key="dbg")
    ph.close()


def host_small(inputs):
    conv_w = inputs["conv_w"][0]
    conv_b = inputs["conv_b"][0]
    gb = np.concatenate([inputs["b_igate"][0], inputs["b_fgate"][0]])
    rep = lambda v: np.ascontiguousarray(np.broadcast_to(v[None, :], (128, v.shape[0]))).astype(np.float32)
    return {
        "s_cwT": np.ascontiguousarray(conv_w.reshape(4, 4, 128).transpose(2, 1, 0)).reshape(128, 16),
        "s_cbT": np.ascontiguousarray(conv_b.reshape(4, 128).T),
        "s_gb": rep(np.tile(gb, 16)),
        "s_mgT": np.ascontiguousarray(inputs["mnorm_g"][0].reshape(4, 128).T),
        "s_ln1g": rep(inputs["ln1_g"][0]), "s_ln1b": rep(inputs["ln1_b"][0]),
        "s_ln2g": rep(inputs["ln2_g"][0]), "s_ln2b": rep(inputs["ln2_b"][0]),
        "s_brt": rep(inputs["b_router"][0]),
        "s_ecap": rep(np.arange(NEXP, dtype=np.float32) * CAP),
    }


def mlstm_phase(nc, c, s, w_in, wmq_d, wmk_d, sm, xT, hmT, dbg=None):
    ph = Phase(nc, f"ml{s}")
    w_in_v = w_in.rearrange("(c p) n -> p c n", p=128)
    wm = ph.sb("wm", [128, 8, 1544], BF16)

    def ldw(cc):
        ph.dma("pool", lambda e: e.dma_start(out=wm[:, cc, :], in_=w_in_v[:, cc, 1536:3080]),
               writes=[("wm", cc)], key=f"wm{cc}")
    for cc in range(8):
        ldw(cc)
    wq = ph.sb("wq", [128, 4, 128], BF16)
    wk = ph.sb("wk", [128, 4, 128], BF16)
    ph.dma("pool", lambda e: e.dma_start(out=wq[:], in_=wmq_d.rearrange("h e f -> e h f")), writes=["wq"], key="wq")
    ph.dma("pool", lambda e: e.dma_start(out=wk[:], in_=wmk_d.rearrange("h e f -> e h f")), writes=["wk"], key="wk")
    cw = ph.sb("cw", [128, 16], F32)
    cb = ph.sb("cb", [128, 4], F32)
    gb = ph.sb("gb", [128, 128], F32)
    mgT = ph.sb("mgT", [128, 4], F32)
    ph.dma("sp", lambda e: e.dma_start(out=cw[:], in_=sm["cwT"]), writes=["cw"], key="cw")
    ph.dma("sp", lambda e: e.dma_start(out=cb[:], in_=sm["cbT"]), writes=["cb"], key="cb")
    ph.dma("sp", lambda e: e.dma_start(out=gb[:], in_=sm["gb"]), writes=["gb"], key="gb")
    ph.dma("sp", lambda e: e.dma_start(out=mgT[:], in_=sm["mgT"]), writes=["mgT"], key="mgT")
    xall = [("xT", cc) for cc in range(8)]

    pj = [ph.ps(f"pj{i}", [128, 512], F32) for i in range(2)]
    misc = ph.ps("misc", [128, 512], F32)
    st = [ph.ps(f"st{i}", [128, 512], F32) for i in range(2)]
    ob = [ph.ps(f"ob{i}", [128, 512], F32) for i in range(2)]
    tpb = ph.ps("tpb", [128, 512], F32)
    cnt = {"pj": 0, "ev": 0, "st": 0, "ob": 0}

    def mm(out, lhsT, rhs, start, stop, reads, writes):
        ph.op("pe", lambda e: e.matmul(out, lhsT=lhsT, rhs=rhs, start=start, stop=stop), reads=reads, writes=writes)

    def nextpj():
        i = cnt["pj"] % 2
        cnt["pj"] += 1
        return pj[i], ("pj", i)

    def evac(out, in_, reads, writes):
        cnt["ev"] += 1
        if cnt["ev"] % 2 == 0:
            ph.op("act", lambda e: e.copy(out=out, in_=in_), reads=reads, writes=writes)
        else:
            ph.op("dve", lambda e: e.tensor_copy(out=out, in_=in_), reads=reads, writes=writes)

    def dve(fn, reads, writes):
        ph.op("dve", fn, reads=reads, writes=writes)

    def act(fn, reads, writes):
        ph.op("act", fn, reads=reads, writes=writes)

    for ck in range(16):
        for cc in range(8):
            mm(misc[:, 8 * ck: 8 * ck + 8], xT[:, cc, 128 * ck: 128 * ck + 128], wm[:, cc, 1536:1544],
               cc == 0, cc == 7, [("xT", cc), ("wm", cc)], ["misc"])
    gsb = ph.sb("gsb", [128, 128], F32)
    dve(lambda e: e.tensor_tensor(out=gsb[:], in0=misc[:, 0:128], in1=gb[:], op=ALU.add), ["misc", "gb"], ["gsb"])
    gsb3 = gsb[:, :].rearrange("p (c g) -> p c g", g=8)
    sp = ph.sb("sp", [128, 16, 4], F32)
    eb = ph.sb("eb", [128, 16, 4], F32)
    eg = ph.sb("eg", [128, 16, 4], F32)
    egl = ph.sb("egl", [128, 16, 4], F32)
    act(lambda e: e.activation(out=sp[:], in_=gsb3[:, :, 4:8], func=AF.Exp, scale=-1.0), ["gsb"], ["sp"])
    act(lambda e: e.activation(out=sp[:], in_=sp[:], func=AF.Ln, bias=1.0), ["sp"], ["sp"])
    spf = sp[:, :, :].rearrange("p c g -> p (c g)")
    mm(misc[:, 128:192], c.tri_f[:, :], spf, True, True, ["sp"], ["misc"])
    mm(misc[:, 192:256], c.ones_f[:, :], spf, True, True, ["sp"], ["misc"])
    ebf = eb[:, :, :].rearrange("p c g -> p (c g)")
    dve(lambda e: e.tensor_tensor(out=eb[:], in0=misc[:, 128:192].rearrange("p (c g) -> p c g", g=4),
                                  in1=gsb3[:, :, 0:4], op=ALU.add), ["misc", "gsb"], ["eb"])
    act(lambda e: e.activation(out=ebf, in_=ebf, func=AF.Exp), ["eb"], ["eb"])
    act(lambda e: e.activation(out=eg[:, :, :].rearrange("p c g -> p (c g)"), in_=misc[:, 128:192], func=AF.Exp,
                               scale=-1.0), ["misc"], ["eg"])
    act(lambda e: e.activation(out=egl[:, :, :].rearrange("p c g -> p (c g)"), in_=misc[:, 192:256], func=AF.Exp,
                               scale=-1.0), ["misc"], ["egl"])

    xmpad = [ph.sb("xmpad0", [128, S + 3], F32)] * 2
    ctmp = [ph.sb("ctmp0", [128, S], F32)] * 2
    xc = [ph.sb(f"xc{i}", [128, S], BF16) for i in range(2)]
    qT = [ph.sb(f"qT{i}", [128, S], BF16) for i in range(2)]
    kT = [ph.sb(f"kT{i}", [128, S], BF16) for i in range(2)]
    ktok = [ph.sb(f"ktok{i}", [128, 16, 128], BF16) for i in range(2)]
    Vs = [ph.sb(f"Vs{i}", [128, 16, 129], BF16) for i in range(2)]
    sgo = [ph.sb(f"sgo{i}", [128, 16, 128], BF16) for i in range(2)]
    Call = [ph.sb("Call0", [128, 16, 129], F32)] * 2
    Cball = [ph.sb(f"Cball{i}", [128, 16, 129], BF16) for i in range(2)]
    pT = [ph.sb("pT0", [128, S], BF16)] * 2
    hm3 = [ph.sb(f"hm3{i}", [128, 3, 128], F32) for i in range(2)]
    junk = [ph.sb("junk0", [128, 128], F32)] * 2
    sm1 = [ph.sb(f"sm1{i}", [128, 20], F32) for i in range(2)]
    ph.op("pool", lambda e: e.memset(xmpad[0][:, 0:3], 0.0), writes=[("xmp", 0)])

    def inproj(h):
        bi = h % 2
        for n in range(4):
            p, ptok = nextpj()
            for cc in range(8):
                mm(p[:, :], wm[:, cc, 128 * h: 128 * h + 128], xT[:, cc, 512 * n: 512 * n + 512], cc == 0, cc == 7,
                   [("xT", cc), ("wm", cc)], [ptok])
            evac(xmpad[bi][:, 3 + 512 * n: 3 + 512 * n + 512], p[:, :], [ptok, ("xmp", 0)], [("xm", 0, n)])
        xmr = [("xm", 0, n) for n in range(4)]
        dve(lambda e: e.tensor_scalar(out=ctmp[bi][:, :], in0=xmpad[bi][:, 0:S], scalar1=cw[:, 4 * h: 4 * h + 1],
                                      scalar2=cb[:, h: h + 1], op0=ALU.mult, op1=ALU.add),
            xmr + ["cw", "cb"], [("ctmp", 0)])
        for j in range(1, 4):
            (lambda j: dve(lambda e: e.scalar_tensor_tensor(out=ctmp[bi][:, :], in0=xmpad[bi][:, j: S + j],
                                                            scalar=cw[:, 4 * h + j: 4 * h + j + 1], in1=ctmp[bi][:, :],
                                                            op0=ALU.mult, op1=ALU.add),
                           xmr + [("ctmp", 0)], [("ctmp", 0)]))(j)
        for c4 in range(4):
            p, ptok = nextpj()
            for i in range(4):
                ck = 4 * c4 + i
                for cc in range(8):
                    mm(p[:, 128 * i: 128 * i + 128], xT[:, cc, 128 * ck: 128 * ck + 128],
                       wm[:, cc, 512 + 128 * h: 512 + 128 * h + 128], cc == 0, cc == 7, [("xT", cc), ("wm", cc)], [ptok])
            for i in range(4):
                ck = 4 * c4 + i
                (lambda p, i, ck: act(lambda e: e.activation(out=Vs[bi][:, ck, 0:128], in_=p[:, 128 * i: 128 * i + 128], func=AF.Copy,
                                                            scale=eb[:, ck, h: h + 1]), [ptok, "eb"], [("Vs", bi)]))(p, i, ck)
        ph.op("pool", lambda e: e.tensor_copy(out=Vs[bi][:, :, 128], in_=eb[:, :, h]), reads=["eb"], writes=[("Vs", bi)])
        for c4 in range(4):
            p, ptok = nextpj()
            for i in range(4):
                ck = 4 * c4 + i
                for cc in range(8):
                    mm(p[:, 128 * i: 128 * i + 128], xT[:, cc, 128 * ck: 128 * ck + 128],
                       wm[:, cc, 1024 + 128 * h: 1024 + 128 * h + 128], cc == 0, cc == 7, [("xT", cc), ("wm", cc)], [ptok])
            (lambda p, c4: act(lambda e: e.activation(out=sgo[bi][:, 4 * c4: 4 * c4 + 4, :],
                                                      in_=p[:, :].rearrange("p (i f) -> p i f", i=4), func=AF.Sigmoid),
                               [ptok], [("sgo", bi)]))(p, c4)
        act(lambda e: e.activation(out=xc[bi][:, :], in_=ctmp[bi][:, :], func=AF.Silu), [("ctmp", 0)], [("xc", bi)])
        for n in range(4):
            p, ptok = nextpj()
            mm(p[:, :], wq[:, h, :], xc[bi][:, 512 * n: 512 * n + 512], True, True, [("xc", bi), "wq"], [ptok])
            (lambda p, n: act(lambda e: e.activation(out=qT[bi][:, 512 * n: 512 * n + 512], in_=p[:, :], func=AF.Copy,
                                                     scale=float(128 ** -0.5)), [ptok], [("qT", bi)]))(p, n)
            p, ptok = nextpj()
            mm(p[:, :], wk[:, h, :], xc[bi][:, 512 * n: 512 * n + 512], True, True, [("xc", bi), "wk"], [ptok])
            evac(kT[bi][:, 512 * n: 512 * n + 512], p[:, :], [ptok], [("kT", bi)])
        for c4 in range(4):
            p, ptok = nextpj()
            for i in range(4):
                ck = 4 * c4 + i
                mm(p[:, 128 * i: 128 * i + 128], xc[bi][:, 128 * ck: 128 * ck + 128], wk[:, h, :], True, True,
                   [("xc", bi), "wk"], [ptok])
            evac(ktok[bi][:, 4 * c4: 4 * c4 + 4, :], p[:, :].rearrange("p (i f) -> p i f", i=4), [ptok], [("ktok", bi)])

    GROUPS = [(0, 3), (3, 3), (6, 3), (9, 3), (12, 3), (15, 1)]

    def stageA(h):
        bi = h % 2
        dve(lambda e: e.memset(Call[bi][:, 0, :], 0.0), [], [("Call", 0, 0)])
        for (c0, n) in GROUPS[:5]:
            p, ptok = nextpj()
            for i in range(n):
                ck = c0 + i
                mm(p[:, 129 * i: 129 * i + 129], ktok[bi][:, ck, :], Vs[bi][:, ck, :], True, True, [("ktok", bi), ("Vs", bi)], [ptok])
            for i in range(n):
                ck = c0 + i
                (lambda p, i, ck: act(lambda e: e.activation(out=Call[bi][:, ck + 1, :], in_=p[:, 129 * i: 129 * i + 129], func=AF.Copy,
                                                             scale=egl[:, ck, h: h + 1]), [ptok, "egl"], [("Call", 0, ck + 1)]))(p, i, ck)
        for ck in range(1, 15):
            (lambda ck: dve(lambda e: e.scalar_tensor_tensor(out=Call[bi][:, ck + 1, :], in0=Call[bi][:, ck, :], scalar=egl[:, ck, h: h + 1],
                                                             in1=Call[bi][:, ck + 1, :], op0=ALU.mult, op1=ALU.add),
                            [("Call", 0, ck), ("Call", 0, ck + 1), "egl"], [("Call", 0, ck + 1)]))(ck)
        act(lambda e: e.copy(out=Cball[bi][:, :, :], in_=Call[bi][:, :, :]), [("Call", 0, ck) for ck in range(16)], [("Cball", bi)])

    def stageB(h):
        bi = h % 2
        for q4 in range(4):
            si = cnt["st"] % 2
            cnt["st"] += 1
            for i in range(4):
                ck = 4 * q4 + i
                csl = slice(128 * ck, 128 * ck + 128)
                mm(st[si][:, 128 * i: 128 * i + 128], kT[bi][:, csl], qT[bi][:, csl], True, True, [("kT", bi), ("qT", bi)], [("st", si)])
            (lambda si, q4: dve(lambda e: e.tensor_tensor(out=pT[bi][:, 512 * q4: 512 * q4 + 512], in0=st[si][:, :], in1=c.tri4_bf[:, :], op=ALU.mult),
                                [("st", si)], [("pT", 0, q4)]))(si, q4)
        for (c0, n) in GROUPS:
            oi = cnt["ob"] % 2
            cnt["ob"] += 1
            o = ob[oi]
            otok = ("ob", oi)
            for i in range(n):
                ck = c0 + i
                csl = slice(128 * ck, 128 * ck + 128)
                mm(o[:, 129 * i: 129 * i + 129], pT[bi][:, csl], Vs[bi][:, ck, :], True, ck == 0, [("pT", 0, ck // 4), ("Vs", bi)], [otok])
                if ck > 0:
                    mm(o[:, 129 * i: 129 * i + 129], qT[bi][:, csl], Cball[bi][:, ck, :], False, True, [("qT", bi), ("Cball", bi)], [otok])
            o3 = o[:, 0: 129 * n].rearrange("p (i f) -> p i f", f=129)
            s1 = sm1[oi]
            eg3 = eg[:, c0: c0 + n, h]

            def grp(o, o3, s1, eg3, c0, n, oi, otok):
                dve(lambda e: e.tensor_tensor(out=s1[:, 0:n], in0=o3[:, :, 128], in1=eg3, op=ALU.mult), [otok, "eg"], [("s1", oi)])
                act(lambda e: e.activation(out=s1[:, 0:n], in_=s1[:, 0:n], func=AF.Abs), [("s1", oi)], [("s1", oi)])
                dve(lambda e: e.tensor_scalar(out=s1[:, 0:n], in0=s1[:, 0:n], scalar1=1.0, scalar2=None, op0=ALU.max), [("s1", oi)], [("s1", oi)])
                dve(lambda e: e.reciprocal(out=s1[:, 0:n], in_=s1[:, 0:n]), [("s1", oi)], [("s1", oi)])
                dve(lambda e: e.tensor_tensor(out=s1[:, 4:4 + n], in0=s1[:, 0:n], in1=eg3, op=ALU.mult), [("s1", oi), "eg"], [("fac", oi)])
                for i in range(n):
                    (lambda i: act(lambda e: e.activation(out=junk[oi][:, :], in_=o[:, 129 * i: 129 * i + 128], func=AF.Square,
                                                          scale=s1[:, 4 + i: 5 + i], accum_out=s1[:, 8 + i: 9 + i]),
                                   [otok, ("fac", oi)], [("ss", oi, i), ("junk", 0)]))(i)
                dve(lambda e: e.tensor_scalar(out=s1[:, 12:12 + n], in0=s1[:, 8:8 + n], scalar1=1.0 / 128.0, scalar2=RMS_EPS,
                                              op0=ALU.mult, op1=ALU.add), [("ss", oi, i) for i in range(n)], [("t3", oi)])
                act(lambda e: e.activation(out=s1[:, 12:12 + n], in_=s1[:, 12:12 + n], func=AF.Sqrt), [("t3", oi)], [("t3", oi)])
                dve(lambda e: e.reciprocal(out=s1[:, 12:12 + n], in_=s1[:, 12:12 + n]), [("t3", oi)], [("t3", oi)])
                dve(lambda e: e.tensor_tensor(out=s1[:, 16:16 + n], in0=s1[:, 12:12 + n], in1=s1[:, 4:4 + n], op=ALU.mult),
                    [("t3", oi), ("fac", oi)], [("sc2", oi)])
                for i in range(n):
                    (lambda i: dve(lambda e: e.scalar_tensor_tensor(out=hm3[oi][:, i, :], in0=o[:, 129 * i: 129 * i + 128], scalar=s1[:, 16 + i: 17 + i],
                                                                    in1=sgo[bi][:, c0 + i, :], op0=ALU.mult, op1=ALU.mult),
                                   [otok, ("sc2", oi), ("sgo", bi)], [("hm3", oi, i)]))(i)
                for i in range(n):
                    (lambda i: ph.op("pe", lambda e: e.transpose(out=tpb[:, 128 * i: 128 * i + 128], in_=hm3[oi][:, i, :], identity=c.ident_f[:, :]),
                                     reads=[("hm3", oi, i)], writes=["tpb"]))(i)
                act(lambda e: e.activation(out=hmT[:, h, 128 * c0: 128 * (c0 + n)], in_=tpb[:, 0: 128 * n], func=AF.Copy, scale=mgT[:, h: h + 1]),
                    ["tpb", "mgT"], [("hmT", h, c0)])
            grp(o, o3, s1, eg3, c0, n, oi, otok)

    inproj(0)
    stageA(0)
    inproj(1)
    stageA(1)
    stageB(0)
    inproj(2)
    stageA(2)
    stageB(1)
    inproj(3)
    stageA(3)
    stageB(2)
    stageB(3)
    if dbg is not None:
        ph.dma("sp", lambda e: e.dma_start(out=dbg, in_=hmT[:]),
               reads=[("hmT", h, c0) for h in range(4) for (c0, n) in GROUPS], key="dbg")
    ph.close()


def outproj_phase(nc, c, s, x_d, w_out, w_router, sm, attnT, hmT, P, h1d, Xg, dbg=None):
    ph = Phase(nc, f"op{s}")
    woA = ph.sb("woA", [64, 8, D], BF16)
    woM = ph.sb("woM", [128, 4, D], BF16)
    woA_v = w_out[0:512, :].rearrange("(h e) n -> e h n", e=64)
    woM_v = w_out[512:1024, :].rearrange("(h f) n -> f h n", f=128)

    def ldw(h):
        ph.dma("pool", lambda e: e.dma_start(out=woA[:, h, :], in_=woA_v[:, h, :]), writes=[("woA", h)], key=f"woA{h}")
        if h < 4:
            ph.dma("pool", lambda e: e.dma_start(out=woM[:, h, :], in_=woM_v[:, h, :]), writes=[("woM", h)], key=f"woM{h}")
    for h in range(8):
        ldw(h)
    lng = ph.sb("lng", [128, D], F32)
    lnb = ph.sb("lnb", [128, D], F32)
    brt = ph.sb("brt", [128, 32], F32)
    ecap = ph.sb("ecap", [128, 32], F32)
    wr = ph.sb("wr", [128, 8, 32], F32)
    ph.dma("sp", lambda e: e.dma_start(out=lng[:], in_=sm["ln1g"]), writes=["lng"], key="lng")
    ph.dma("sp", lambda e: e.dma_start(out=lnb[:], in_=sm["ln1b"]), writes=["lnb"], key="lnb")
    ph.dma("sp", lambda e: e.dma_start(out=brt[:], in_=sm["brt"]), writes=["brt"], key="brt")
    ph.dma("sp", lambda e: e.dma_start(out=ecap[:], in_=sm["ecap"]), writes=["ecap"], key="ecap")
    ph.dma("sp", lambda e: e.dma_start(out=wr[:], in_=w_router.rearrange("(c p) n -> p c n", p=128)), writes=["wr"], key="wr")
    if s == 0:
        ph.op("dve", lambda e: e.memset(P.base[:], 0.0), writes=["base"])
        if dbg is not None:
            ph.op("pool", lambda e: e.memset(P.rows[:], 0), writes=[("rows", t) for t in range(32)])
            ph.op("pool", lambda e: e.memset(P.gates[:], 0.0), writes=[("gates", t) for t in range(32)])
    woAr = [("woA", h) for h in range(8)]
    woMr = [("woM", h) for h in range(4)]

    yp = [ph.ps(f"yp{i}", [128, D], F32) for i in range(2)]
    tp = ph.ps("tp", [128, D], F32)
    lgp = ph.ps("lgp", [128, 32], F32)
    pfp = ph.ps("pfp", [128, 64], F32)
    xt = [ph.sb(f"xt{i}", [128, D], F32) for i in range(2)]
    r = [ph.sb(f"r{i}", [128, D], F32) for i in range(2)]
    h1 = [ph.sb(f"h1{i}", [128, D], F32) for i in range(2)]
    h1b = [ph.sb(f"h1b{i}", [128, D], BF16) for i in range(2)]
    h1T = [ph.sb(f"h1T{i}", [128, 8, 128], F32) for i in range(2)]
    st6 = [ph.sb(f"st6{i}", [128, 2, 6], F32) for i in range(2)]
    sv = [ph.sb(f"sv{i}", [128, 16], F32) for i in range(2)]
    lg = [ph.sb(f"lg{i}", [128, 32], F32) for i in range(2)]
    lgraw = [ph.sb(f"lgraw{i}", [128, 32], F32) for i in range(2)]
    t8 = [ph.sb(f"t8{i}", [128, 8], F32) for i in range(2)]
    e4 = [ph.sb(f"e4{i}", [128, 4], F32) for i in range(2)]
    Mf = [ph.sb(f"Mf{i}", [128, 32], F32) for i in range(2)]
    Mb = [ph.sb(f"Mb{i}", [128, 32], BF16) for i in range(2)]
    ex = [ph.sb(f"ex{i}", [128, 32], F32) for i in range(2)]
    rowf = [ph.sb(f"rowf{i}", [128, 32], F32) for i in range(2)]
    j32 = [ph.sb(f"j32{i}", [128, 32], F32) for i in range(2)]
    rk = [ph.sb(f"rk{i}", [128, 4], F32) for i in range(2)]

    def mm(out, lhsT, rhs, start, stop, reads, writes):
        ph.op("pe", lambda e: e.matmul(out, lhsT=lhsT, rhs=rhs, start=start, stop=stop), reads=reads, writes=writes)

    def dve(fn, reads, writes):
        ph.op("dve", fn, reads=reads, writes=writes)

    def act(fn, reads, writes):
        ph.op("act", fn, reads=reads, writes=writes)

    def pool(fn, reads, writes):
        ph.op("pool", fn, reads=reads, writes=writes)

    def stage1(tt):
        gt = 16 * s + tt
        b = tt % 2
        tsl = slice(128 * tt, 128 * tt + 128)
        ph.dma("sp", lambda e: e.dma_start(out=xt[b][:], in_=x_d[128 * gt: 128 * gt + 128, :]), writes=[("xt", b)], key=f"xt{b}")
        for nh in range(2):
            nsl = slice(512 * nh, 512 * nh + 512)
            for h in range(8):
                mm(yp[b][:, nsl], attnT[:, h, tsl], woA[:, h, nsl], h == 0, False, [("attnT", h), ("woA", h)], [("yp", b, nh)])
            for h in range(4):
                mm(yp[b][:, nsl], hmT[:, h, tsl], woM[:, h, nsl], False, h == 3, [("hmT", h), ("woM", h)], [("yp", b, nh)])

    def tile(tt):
        gt = 16 * s + tt
        b = tt % 2
        tsl = slice(128 * tt, 128 * tt + 128)
        for nh in range(2):
            nsl = slice(512 * nh, 512 * nh + 512)
            (lambda nsl, nh: dve(lambda e: e.scalar_tensor_tensor(out=r[b][:, nsl], in0=xt[b][:, nsl], scalar=float(ALPHA),
                                                                  in1=yp[b][:, nsl], op0=ALU.mult, op1=ALU.add),
                                 [("xt", b), ("yp", b, nh)], [("r", b, nh)]))(nsl, nh)
            (lambda nsl, nh: dve(lambda e: e.bn_stats(out=st6[b][:, nh, :], in_=r[b][:, nsl]), [("r", b, nh)], [("st6", b, nh)]))(nsl, nh)
        svb = sv[b]
        dve(lambda e: e.bn_aggr(out=svb[:, 0:2], in_=st6[b][:, :, :].rearrange("p a b -> p (a b)")),
            [("st6", b, 0), ("st6", b, 1)], [("sv", b)])
        dve(lambda e: e.tensor_scalar(out=svb[:, 2:3], in0=svb[:, 1:2], scalar1=float(LN_EPS), scalar2=None, op0=ALU.add),
            [("sv", b)], [("sv2", b)])
        act(lambda e: e.activation(out=svb[:, 2:3], in_=svb[:, 2:3], func=AF.Ln), [("sv2", b)], [("sv2", b)])
        act(lambda e: e.activation(out=svb[:, 2:3], in_=svb[:, 2:3], func=AF.Exp, scale=-0.5), [("sv2", b)], [("sv2", b)])
        dve(lambda e: e.tensor_scalar(out=svb[:, 6:7], in0=svb[:, 0:1], scalar1=-1.0, scalar2=None, op0=ALU.mult), [("sv", b)], [("nm", b)])
        act(lambda e: e.activation(out=r[b][:, :], in_=r[b][:, :], func=AF.Identity, bias=svb[:, 6:7]),
            [("r", b, 0), ("r", b, 1), ("nm", b), ("st6", b, 0), ("st6", b, 1)], [("r", b, 0), ("r", b, 1)])
        dve(lambda e: e.scalar_tensor_tensor(out=h1[b][:, :], in0=r[b][:, :], scalar=svb[:, 2:3], in1=lng[:, :],
                                             op0=ALU.mult, op1=ALU.mult), [("r", b, 0), ("r", b, 1), ("sv2", b), "lng"], [("h1", b)])
        pool(lambda e: e.tensor_tensor(out=h1[b][:, :], in0=h1[b][:, :], in1=lnb[:, :], op=ALU.add), [("h1", b), "lnb"], [("h1", b)])
        ph.dma("sp", lambda e: e.dma_start(out=h1d[128 * gt: 128 * gt + 128, :], in_=h1[b][:, :]), reads=[("h1", b)], key=f"h1o{b}")
        for cc in range(8):
            ph.op("pe", (lambda cc: lambda e: e.transpose(out=tp[:, 128 * cc: 128 * cc + 128], in_=h1[b][:, 128 * cc: 128 * cc + 128],
                                                          identity=c.ident_f[:, :]))(cc),
                  reads=[("h1", b)], writes=[("tp", cc // 4)])

    def tileB2(tt):
        gt = 16 * s + tt
        b = tt % 2
        act(lambda e: e.copy(out=h1b[b][:, :], in_=h1[b][:, :]), [("h1", b)], [("h1b", b)])
        act(lambda e: e.copy(out=h1T[b][:, 0:4, :], in_=tp[:, 0:512].rearrange("p (c t) -> p c t", c=4)), [("tp", 0)], [("h1T", b, 0)])
        act(lambda e: e.copy(out=h1T[b][:, 4:8, :], in_=tp[:, 512:1024].rearrange("p (c t) -> p c t", c=4)), [("tp", 1)], [("h1T", b, 1)])
        for cc in range(8):
            mm(lgp[:, :], h1T[b][:, cc, :], wr[:, cc, :], cc == 0, cc == 7, [("h1T", b, cc // 4), "wr"], ["lgp"])
        act(lambda e: e.copy(out=lgraw[b][:, :], in_=lgp[:, :]), ["lgp"], [("lgraw", b)])

    def stageC(tt):
        gt = 16 * s + tt
        b = tt % 2
        svb = sv[b]
        dve(lambda e: e.tensor_tensor(out=lg[b][:, :], in0=lgraw[b][:, :], in1=brt[:, :], op=ALU.add), [("lgraw", b), "brt"], [("lg", b)])
        dve(lambda e: e.max(out=t8[b][:, :], in_=lg[b][:, :]), [("lg", b)], [("t8", b)])
        dve(lambda e: e.tensor_scalar(out=svb[:, 3:4], in0=t8[b][:, 0:1], scalar1=-1.0, scalar2=None, op0=ALU.mult), [("t8", b)], [("nt0", b)])
        act(lambda e: e.activation(out=e4[b][:, :], in_=t8[b][:, 0:4], func=AF.Exp, bias=svb[:, 3:4], accum_out=svb[:, 4:5]),
            [("t8", b), ("nt0", b)], [("e4", b), ("gsum", b)])
        dve(lambda e: e.reciprocal(out=svb[:, 5:6], in_=svb[:, 4:5]), [("gsum", b)], [("rs", b)])
        dve(lambda e: e.tensor_scalar(out=P.gates[:, gt, :], in0=e4[b][:, :], scalar1=svb[:, 5:6], scalar2=None, op0=ALU.mult),
            [("e4", b), ("rs", b)], [("gates", gt)])
        dve(lambda e: e.tensor_scalar(out=Mf[b][:, :], in0=lg[b][:, :], scalar1=t8[b][:, 3:4], scalar2=None, op0=ALU.is_ge),
            [("lg", b), ("t8", b)], [("Mf", b)])
        dve(lambda e: e.tensor_copy(out=Mb[b][:, :], in_=Mf[b][:, :]), [("Mf", b)], [("Mb", b)])
        act(lambda e: e.activation(out=ex[b][:, :], in_=lg[b][:, :], func=AF.Exp, bias=svb[:, 3:4]), [("lg", b), ("nt0", b)], [("ex", b)])
        dve(lambda e: e.scalar_tensor_tensor(out=P.Gd[:, gt, :], in0=ex[b][:, :], scalar=svb[:, 5:6], in1=Mf[b][:, :],
                                             op0=ALU.mult, op1=ALU.mult), [("ex", b), ("rs", b), ("Mf", b)], [("Gd", gt)])
        mm(pfp[:, 0:32], c.tris_bf[:, :], Mb[b][:, :], True, True, [("Mb", b)], ["pfp"])
        mm(pfp[:, 32:64], c.ones_bf[:, :], Mb[b][:, :], True, True, [("Mb", b)], ["pfp"])
        dve(lambda e: e.tensor_tensor(out=rowf[b][:, :], in0=pfp[:, 0:32], in1=P.base[:, :], op=ALU.add), ["pfp", "base"], [("rowf", b)])
        dve(lambda e: e.tensor_tensor(out=P.base[:, :], in0=pfp[:, 32:64], in1=P.base[:, :], op=ALU.add), ["pfp", "base"], ["base"])
        dve(lambda e: e.tensor_scalar(out=rowf[b][:, :], in0=rowf[b][:, :], scalar1=float(CAP - 1), scalar2=None, op0=ALU.min),
            [("rowf", b)], [("rowf", b)])
        dve(lambda e: e.tensor_tensor(out=rowf[b][:, :], in0=rowf[b][:, :], in1=ecap[:, :], op=ALU.add), [("rowf", b), "ecap"], [("rowf", b)])
        for k in range(4):
            (lambda k: dve(lambda e: e.scalar_tensor_tensor(out=j32[b][:, :], in0=lg[b][:, :], scalar=t8[b][:, k: k + 1], in1=rowf[b][:, :],
                                                            op0=ALU.is_equal, op1=ALU.mult, accum_out=rk[b][:, k: k + 1]),
                           [("lg", b), ("t8", b), ("rowf", b)], [("j32", b), ("rk", b, k)]))(k)
        dve(lambda e: e.tensor_copy(out=P.rows[:, gt, :], in_=rk[b][:, :]), [("rk", b, k) for k in range(4)], [("rows", gt)])
        for k in range(4):
            (lambda k: ph.dma("pool", lambda e: e.indirect_dma_start(
                out=Xg, out_offset=bass.IndirectOffsetOnAxis(ap=P.rows[:, gt, k: k + 1], axis=0), in_=h1b[b][:, :], in_offset=None),
                reads=[("rows", gt), ("h1b", b)], key=f"sc{b}_{k}"))(k)

    stage1(0)
    stage1(1)
    tile(0)
    tileB2(0)
    for tt in range(16):
        if tt + 2 < 16:
            stage1(tt + 2)
        if tt + 1 < 16:
            tile(tt + 1)
        stageC(tt)
        if tt + 1 < 16:
            tileB2(tt + 1)
    if dbg is not None:
        ph.dma("sp", lambda e: e.dma_start(out=dbg["rows"], in_=P.rows[:]), reads=[("rows", t) for t in range(32)], key="dbgr")
        ph.dma("sp", lambda e: e.dma_start(out=dbg["gates"], in_=P.gates[:]), reads=[("gates", t) for t in range(32)], key="dbgg")
    ph.close()


NROWS = NEXP * CAP


class Persist:
    pass


def build_program(stop_after=None, debug=False, nseq=NSEQ):
    nc = bass.Bass("TRN2", target_bir_lowering=False)
    dt = lambda name, shape, dtype, kind: nc.dram_tensor(name, list(shape), dtype, kind=kind).ap()
    xT_d = dt("xT", [NSEQ, D, S], F32, "ExternalInput")
    x_d = dt("x", [TOK, D], F32, "ExternalInput")
    w_in = dt("w_in", [D, PW], F32, "ExternalInput")
    wmq = dt("w_mq", [4, 128, 128], F32, "ExternalInput")
    wmk = dt("w_mk", [4, 128, 128], F32, "ExternalInput")
    w_out = dt("w_out", [D, D], F32, "ExternalInput")
    w_router = dt("w_router", [D, NEXP], F32, "ExternalInput")
    hc = host_consts()
    cd = {k[2:]: dt(k, v.shape, F32, "ExternalInput") for k, v in hc.items()}
    sm = {k[2:]: dt(k, shp, F32, "ExternalInput") for k, shp in SMALL_SHAPES.items()}
    wg_d = dt("w_gate", [NEXP, D, D], F32, "ExternalInput")
    wu_d = dt("w_up", [NEXP, D, D], F32, "ExternalInput")
    wd_d = dt("w_down", [NEXP, D, D], F32, "ExternalInput")
    bgT_d = dt("bgT", [128, NEXP * 8], F32, "ExternalInput")
    buT_d = dt("buT", [128, NEXP * 8], F32, "ExternalInput")
    bd_d = dt("b_down", [NEXP, D], F32, "ExternalInput")
    out_d = dt("out", [TOK, D], F32, "ExternalOutput")
    Yg = dt("Yg", [NROWS, D], F32, "Internal")
    scratch_kind = "ExternalOutput" if debug else "Internal"
    h1d = dt("h1d", [TOK, D], F32, scratch_kind)
    Xg = dt("Xg", [NROWS, D], BF16, scratch_kind)
    dbg = None
    if debug:
        dbg = {"rows": dt("dbg_rows", [128, 32, 4], I32, "ExternalOutput"),
               "gates": dt("dbg_gates", [128, 32, 4], F32, "ExternalOutput")}
    top = contextlib.ExitStack()
    c = setup_consts(nc, top, cd)
    P = Persist()
    P.rows = top.enter_context(nc.sbuf_tensor("P_rows", [128, 32, 4], I32))
    P.gates = top.enter_context(nc.sbuf_tensor("P_gates", [128, 32, 4], F32))
    P.Gd = top.enter_context(nc.sbuf_tensor("P_Gd", [128, 32, 32], F32))
    P.base = top.enter_context(nc.sbuf_tensor("P_base", [128, 32], F32))
    for s in range(nseq):
        with contextlib.ExitStack() as sq:
            xT = sq.enter_context(nc.sbuf_tensor(f"xT_sb{s}", [128, 8, S], BF16))
            attnT = sq.enter_context(nc.sbuf_tensor(f"attnT{s}", [64, 8, S], BF16))
            hmT = sq.enter_context(nc.sbuf_tensor(f"hmT{s}", [128, 4, S], BF16))
            attention_phase(nc, c, s, xT_d, w_in, xT, attnT, Xg=(Xg if s == 0 else None))
            mlstm_phase(nc, c, s, w_in, wmq, wmk, sm, xT, hmT)
            outproj_phase(nc, c, s, x_d, w_out, w_router, sm, attnT, hmT, P, h1d, Xg, dbg=dbg)
    if stop_after == "mixer":
        final_cleanup(nc)
        top.close()
        return nc
    experts_phase(nc, c, wg_d, wu_d, wd_d, bgT_d, buT_d, Xg, Yg)
    combine_phase(nc, c, sm, bd_d, P, h1d, Yg, out_d)
    final_cleanup(nc)
    top.close()
    return nc


SMALL_SHAPES = {"s_cwT": (128, 16), "s_cbT": (128, 4), "s_gb": (128, 128), "s_mgT": (128, 4),
                "s_ln1g": (128, D), "s_ln1b": (128, D), "s_ln2g": (128, D), "s_ln2b": (128, D),
                "s_brt": (128, 32), "s_ecap": (128, 32)}


def core_inputs(inputs, core):
    x = inputs["x"][2 * core: 2 * core + 2]
    m = {
        "xT": np.ascontiguousarray(x.transpose(0, 2, 1)),
        "x": np.ascontiguousarray(x.reshape(TOK, D)),
        "w_in": inputs["w_in"][0], "w_mq": inputs["w_mq"][0], "w_mk": inputs["w_mk"][0],
        "w_out": inputs["w_out"][0], "w_router": inputs["w_router"][0],
        "w_gate": inputs["w_gate"][0], "w_up": inputs["w_up"][0], "w_down": inputs["w_down"][0],
        "b_down": inputs["b_down"][0],
    }
    return m


def host_shared(inputs):
    m = dict(host_consts())
    m.update(host_small(inputs))
    tb = lambda b: np.ascontiguousarray(b.reshape(NEXP, 8, 128).transpose(2, 0, 1)).reshape(128, NEXP * 8)
    m["bgT"] = tb(inputs["b_gate"][0])
    m["buT"] = tb(inputs["b_up"][0])
    return m


_NC_CACHE = {}


def kernel(**inputs):
    inputs = {k: np.asarray(v) for k, v in inputs.items()}
    if "nc" not in _NC_CACHE:
        _NC_CACHE["nc"] = build_program()
    nc = _NC_CACHE["nc"]
    shared = host_shared(inputs)
    in_maps = []
    for core in range(8):
        m = core_inputs(inputs, core)
        m.update(shared)
        in_maps.append(m)
    res = run_bass_kernel_spmd(nc, in_maps, core_ids=list(range(8)))
    out = np.concatenate([r["out"].reshape(NSEQ, S, D) for r in res.results], axis=0)
    return out.astype(np.float32)


def experts_phase(nc, c, wg_d, wu_d, wd_d, bgT_d, buT_d, Xg, Yg, nexp=NEXP):
    ph = Phase(nc, "ex")
    W = [[ph.sb(f"W{i}_{m}", [128, 8, D], BF16) for m in range(3)] for i in range(2)]
    wsrc = [wg_d, wu_d, wd_d]
    bg = ph.sb("bg", [128, NEXP * 8], F32)
    bu1 = ph.sb("bu1", [128, NEXP * 8], F32)
    ph.dma("sp", lambda e: e.dma_start(out=bg[:], in_=bgT_d), writes=["bg"], key="bg")
    ph.dma("sp", lambda e: e.dma_start(out=bu1[:], in_=buT_d), writes=["bu1"], key="bu1")
    ph.op("dve", lambda e: e.tensor_scalar(out=bu1[:], in0=bu1[:], scalar1=1.0, scalar2=None, op0=ALU.add), reads=["bu1"], writes=["bu1"])
    xg = [ph.sb(f"xg{i}", [128, 5, D], BF16) for i in range(2)]
    XgT = [ph.sb(f"XgT{i}", [128, 8, CAP], BF16) for i in range(2)]
    HT = [ph.sb(f"HT{i}", [128, 8, CAP], BF16) for i in range(2)]
    g1 = [ph.sb(f"g1{i}", [128, 384], F32) for i in range(2)]
    sg = [ph.sb(f"sg{i}", [128, 384], F32) for i in range(2)]
    tu = [ph.sb(f"tu{i}", [128, 384], F32) for i in range(2)]
    ysb = [ph.sb(f"ysb{i}", [128, 512], F32) for i in range(4)]
    tpp = [ph.ps(f"tpp{i}", [128, 1024], BF16) for i in range(2)]
    gp = [ph.ps(f"gp{i}", [128, 384], F32) for i in range(2)]
    up = [ph.ps(f"up{i}", [128, 384], F32) for i in range(2)]
    yp = [ph.ps(f"yp{i}", [128, 512], F32) for i in range(2)]
    cnt = {"tp": 0, "gu": 0, "y": 0, "ys": 0, "ev": 0}

    def mm(out, lhsT, rhs, start, stop, reads, writes):
        ph.op("pe", lambda e: e.matmul(out, lhsT=lhsT, rhs=rhs, start=start, stop=stop), reads=reads, writes=writes)

    def dve(fn, reads, writes):
        ph.op("dve", fn, reads=reads, writes=writes)

    def act(fn, reads, writes):
        ph.op("act", fn, reads=reads, writes=writes)

    def load_w(ex):
        wi = ex % 2
        for m in range(3):
            src = wsrc[m][ex].rearrange("(c p) n -> p c n", p=128)
            for cc in range(8):
                (lambda m, cc, src: ph.dma("pool", lambda e: e.dma_start(out=W[wi][m][:, cc, :], in_=src[:, cc, :]),
                                           writes=[("W", wi, m, cc)], key=f"W{wi}_{m}_{cc}"))(m, cc, src)

    def load_x(ex):
        xi = ex % 2
        ph.dma("sp", lambda e: e.dma_start(out=xg[xi][:], in_=Xg[ex * CAP:(ex + 1) * CAP, :].rearrange("(j p) d -> p j d", p=128)),
               writes=[("xg", xi)], key=f"xg{xi}")

    def transposes(ex):
        xi = ex % 2
        for fc in range(8):
            ti = cnt["tp"] % 2
            cnt["tp"] += 1
            for j in range(5):
                (lambda fc, j, ti: ph.op("pe", lambda e: e.transpose(out=tpp[ti][:, 128 * j: 128 * j + 128],
                                                                     in_=xg[xi][:, j, 128 * fc: 128 * fc + 128],
                                                                     identity=c.ident_bf[:, :]),
                                         reads=[("xg", xi)], writes=[("tpp", ti)]))(fc, j, ti)
            cnt["ev"] += 1
            if cnt["ev"] % 2 == 0:
                (lambda fc, ti: act(lambda e: e.copy(out=XgT[xi][:, fc, :], in_=tpp[ti][:, 0:CAP]), [("tpp", ti)], [("XgT", xi, fc)]))(fc, ti)
            else:
                (lambda fc, ti: dve(lambda e: e.tensor_copy(out=XgT[xi][:, fc, :], in_=tpp[ti][:, 0:CAP]), [("tpp", ti)], [("XgT", xi, fc)]))(fc, ti)

    def mm1(ex):
        wi = ex % 2
        xi = ex % 2
        for (p0, n) in ((0, 384), (384, 256)):
            psl = slice(p0, p0 + n)
            for fc in range(8):
                gi = cnt["gu"] % 2
                cnt["gu"] += 1
                fsl = slice(128 * fc, 128 * fc + 128)
                for kc in range(8):
                    mm(gp[gi][:, 0:n], W[wi][0][:, kc, fsl], XgT[xi][:, kc, psl], kc == 0, kc == 7,
                       [("W", wi, 0, kc), ("XgT", xi, kc)], [("gp", gi)])
                for kc in range(8):
                    mm(up[gi][:, 0:n], W[wi][1][:, kc, fsl], XgT[xi][:, kc, psl], kc == 0, kc == 7,
                       [("W", wi, 1, kc), ("XgT", xi, kc)], [("up", gi)])

                def epi(gi, fc, n, psl):
                    bcol = slice(ex * 8 + fc, ex * 8 + fc + 1)
                    dve(lambda e: e.tensor_scalar(out=g1[gi][:, 0:n], in0=gp[gi][:, 0:n], scalar1=bg[:, bcol], scalar2=7.0,
                                                  op0=ALU.add, op1=ALU.min), [("gp", gi), "bg"], [("g1", gi)])
                    act(lambda e: e.activation(out=sg[gi][:, 0:n], in_=g1[gi][:, 0:n], func=AF.Gelu_apprx_sigmoid),
                        [("g1", gi)], [("sg", gi)])
                    dve(lambda e: e.tensor_scalar(out=tu[gi][:, 0:n], in0=up[gi][:, 0:n], scalar1=bu1[:, bcol], scalar2=-6.0,
                                                  op0=ALU.add, op1=ALU.max), [("up", gi), "bu1"], [("tu", gi)])
                    dve(lambda e: e.scalar_tensor_tensor(out=HT[xi][:, fc, psl], in0=tu[gi][:, 0:n], scalar=8.0, in1=sg[gi][:, 0:n],
                                                         op0=ALU.min, op1=ALU.mult), [("tu", gi), ("sg", gi)], [("HT", xi, fc)])
                epi(gi, fc, n, psl)

    def mm2(ex):
        wi = ex % 2
        xi = ex % 2
        for j in range(5):
            for nh in range(2):
                yi = cnt["y"] % 2
                cnt["y"] += 1
                for fc in range(8):
                    mm(yp[yi][:, :], HT[xi][:, fc, 128 * j: 128 * j + 128], W[wi][2][:, fc, 512 * nh: 512 * nh + 512],
                       fc == 0, fc == 7, [("HT", xi, fc), ("W", wi, 2, fc)], [("yp", yi)])
                si = cnt["ys"] % 4
                cnt["ys"] += 1
                cnt["ev"] += 1
                if cnt["ev"] % 2 == 0:
                    (lambda yi, si: act(lambda e: e.copy(out=ysb[si][:, :], in_=yp[yi][:, :]), [("yp", yi)], [("ysb", si)]))(yi, si)
                else:
                    (lambda yi, si: dve(lambda e: e.tensor_copy(out=ysb[si][:, :], in_=yp[yi][:, :]), [("yp", yi)], [("ysb", si)]))(yi, si)
                r0 = ex * CAP + 128 * j
                (lambda si, r0, nh: ph.dma("sp", lambda e: e.dma_start(out=Yg[r0: r0 + 128, 512 * nh: 512 * nh + 512], in_=ysb[si][:, :]),
                                           reads=[("ysb", si)], key=f"ys{si}"))(si, r0, nh)

    load_w(0)
    load_x(0)
    transposes(0)
    for ex in range(nexp):
        if ex + 1 < nexp:
            load_w(ex + 1)
            load_x(ex + 1)
        mm1(ex)
        if ex + 1 < nexp:
            transposes(ex + 1)
        mm2(ex)
    ph.close()


def combine_phase(nc, c, sm, bd_d, P, h1d, Yg, out_d, ntiles=32):
    ph = Phase(nc, "cb")
    lng = ph.sb("lng", [128, D], F32)
    lnb = ph.sb("lnb", [128, D], F32)
    bd = ph.sb("bd", [32, D], F32)
    ph.dma("sp", lambda e: e.dma_start(out=lng[:], in_=sm["ln2g"]), writes=["lng"], key="lng")
    ph.dma("sp", lambda e: e.dma_start(out=lnb[:], in_=sm["ln2b"]), writes=["lnb"], key="lnb")
    ph.dma("sp", lambda e: e.dma_start(out=bd[:], in_=bd_d), writes=["bd"], key="bd")
    yk = [[ph.sb(f"yk{i}_{k}", [128, D], F32) for k in range(4)] for i in range(3)]
    h1 = [ph.sb(f"h1{i}", [128, D], F32) for i in range(3)]
    acc = [ph.sb(f"acc{i}", [128, D], F32) for i in range(2)]
    tmp = [ph.sb(f"tmp{i}", [128, D], F32) for i in range(2)]
    gdT = [ph.sb(f"gdT{i}", [32, 128], F32) for i in range(2)]
    st6 = [ph.sb(f"st6{i}", [128, 2, 6], F32) for i in range(2)]
    sv = [ph.sb(f"sv{i}", [128, 4], F32) for i in range(2)]
    gtp = ph.ps("gtp", [32, 128], F32)
    bp = [ph.ps(f"bp{i}", [128, D], F32) for i in range(2)]

    def mm(out, lhsT, rhs, start, stop, reads, writes):
        ph.op("pe", lambda e: e.matmul(out, lhsT=lhsT, rhs=rhs, start=start, stop=stop), reads=reads, writes=writes)

    def dve(fn, reads, writes):
        ph.op("dve", fn, reads=reads, writes=writes)

    def act(fn, reads, writes):
        ph.op("act", fn, reads=reads, writes=writes)

    def pool(fn, reads, writes):
        ph.op("pool", fn, reads=reads, writes=writes)

    dg = [[ph.sb(f"dg{i}_{k}", [128, 128], F32) for k in range(4)] for i in range(2)]

    def load(gt):
        b = gt % 3
        for k in range(4):
            (lambda k: ph.dma("pool", lambda e: e.indirect_dma_start(
                out=yk[b][k][:, :], out_offset=None, in_=Yg,
                in_offset=bass.IndirectOffsetOnAxis(ap=P.rows[:, gt, k: k + 1], axis=0)),
                writes=[("yk", b, k)], key=f"yk{b}_{k}"))(k)
        ph.dma("sp", lambda e: e.dma_start(out=h1[b][:, :], in_=h1d[128 * gt: 128 * gt + 128, :]), writes=[("h1", b)], key=f"h1{b}")

    def front(gt):
        b = gt % 2
        b3 = gt % 3
        for k in range(3):
            (lambda k: dve(lambda e: e.tensor_scalar(out=dg[b][k][:, :], in0=c.ident_f[:, :], scalar1=P.gates[:, gt, k: k + 1], scalar2=None,
                                                     op0=ALU.mult), [], [("dg", b, k)]))(k)
        act(lambda e: e.activation(out=yk[b3][3][:, :], in_=yk[b3][3][:, :], func=AF.Copy, scale=P.gates[:, gt, 3:4]), [("yk", b3, 3)], [("yk", b3, 3)])
        dve(lambda e: e.scalar_tensor_tensor(out=h1[b3][:, :], in0=h1[b3][:, :], scalar=float(ALPHA), in1=yk[b3][3][:, :],
                                             op0=ALU.mult, op1=ALU.add), [("h1", b3), ("yk", b3, 3)], [("h1", b3)])
        ph.op("pe", lambda e: e.transpose(out=gtp[:, :], in_=P.Gd[:, gt, :], identity=c.ident_f[:, :]), reads=[], writes=["gtp"])
        act(lambda e: e.copy(out=gdT[b][:, :], in_=gtp[:, :]), ["gtp"], [("gdT", b)])
        for nh in range(2):
            nsl = slice(512 * nh, 512 * nh + 512)
            mm(bp[b][:, nsl], gdT[b][:, :], bd[:, nsl], True, False, [("gdT", b), "bd"], [("bp", b, nh)])
            for k in range(3):
                mm(bp[b][:, nsl], dg[b][k][:, :], yk[b3][k][:, nsl], False, k == 2, [("dg", b, k), ("yk", b3, k)], [("bp", b, nh)])

    def back(gt):
        b = gt % 2
        b3 = gt % 3
        for nh in range(2):
            nsl = slice(512 * nh, 512 * nh + 512)
            (lambda nsl, nh: dve(lambda e: e.tensor_tensor(out=acc[b][:, nsl], in0=h1[b3][:, nsl], in1=bp[b][:, nsl], op=ALU.add),
                                 [("h1", b3), ("bp", b, nh)], [("acc", b, nh)]))(nsl, nh)
            (lambda nh: dve(lambda e: e.bn_stats(out=st6[b][:, nh, :], in_=acc[b][:, 512 * nh: 512 * nh + 512]), [("acc", b, nh)], [("st6", b, nh)]))(nh)
        accr = [("acc", b, 0), ("acc", b, 1)]
        svb = sv[b]
        dve(lambda e: e.bn_aggr(out=svb[:, 0:2], in_=st6[b][:, :, :].rearrange("p a b -> p (a b)")), [("st6", b, 0), ("st6", b, 1)], [("sv", b)])
        dve(lambda e: e.tensor_scalar(out=svb[:, 2:3], in0=svb[:, 1:2], scalar1=float(LN_EPS), scalar2=None, op0=ALU.add), [("sv", b)], [("sv2", b)])
        act(lambda e: e.activation(out=svb[:, 2:3], in_=svb[:, 2:3], func=AF.Ln), [("sv2", b)], [("sv2", b)])
        act(lambda e: e.activation(out=svb[:, 2:3], in_=svb[:, 2:3], func=AF.Exp, scale=-0.5), [("sv2", b)], [("sv2", b)])
        dve(lambda e: e.tensor_scalar(out=svb[:, 3:4], in0=svb[:, 0:1], scalar1=-1.0, scalar2=None, op0=ALU.mult), [("sv", b)], [("nm", b)])
        act(lambda e: e.activation(out=acc[b][:, :], in_=acc[b][:, :], func=AF.Identity, bias=svb[:, 3:4]),
            accr + [("nm", b), ("st6", b, 0), ("st6", b, 1)], accr)
        dve(lambda e: e.scalar_tensor_tensor(out=tmp[b][:, :], in0=acc[b][:, :], scalar=svb[:, 2:3], in1=lng[:, :],
                                             op0=ALU.mult, op1=ALU.mult), accr + [("sv2", b), "lng"], [("tmp", b)])
        pool(lambda e: e.tensor_tensor(out=tmp[b][:, :], in0=tmp[b][:, :], in1=lnb[:, :], op=ALU.add), [("tmp", b), "lnb"], [("tmp", b)])
        ph.dma("sp", lambda e: e.dma_start(out=out_d[128 * gt: 128 * gt + 128, :], in_=tmp[b][:, :]), reads=[("tmp", b)], key=f"out{b}")

    load(0)
    if ntiles > 1:
        load(1)
    front(0)
    for gt in range(ntiles):
        if gt + 2 < ntiles:
            load(gt + 2)
        if gt + 1 < ntiles:
            front(gt + 1)
        back(gt)
    ph.close()
```
